# Optimizing a Trainium2 kernel written in Bass

```python
import math
import jax
import jax.numpy as jnp
from jax import lax
import numpy as np

D_MODEL = 2048
BATCH = 4
SEQ = 4096
DEPTH = 4

N_MIXERS = 3
NORM_EPS = 1e-6
MEM_TOKENS = 256
MEM_HEADS = 4
MEM_HEAD_DIM = D_MODEL // 16
MEM_WIDTH = MEM_HEADS * MEM_HEAD_DIM
MIX_WIDTH = D_MODEL - MEM_WIDTH
RET_HEADS = 6
RET_HEAD_DIM = MIX_WIDTH // RET_HEADS
RET_CHUNK = 128
CONV_WIDTH = 3
MOBA_HEADS = 12
MOBA_HEAD_DIM = MIX_WIDTH // MOBA_HEADS
MOBA_BLOCK = 256
MOBA_TOPK = 3
MOBA_Q_CHUNK = 16
D_FF = ((8 * D_MODEL // 3 + 255) // 256) * 256
MIXER_COLS = (4 * MIX_WIDTH, 3 * MIX_WIDTH, 3 * MIX_WIDTH)

kernel_name = 'hybrid_retention_shortconv_moba_trunk'


def rms_norm(x, gain):
    xf = x.astype(jnp.float32)
    y = xf * lax.rsqrt(jnp.mean(xf * xf, axis=-1, keepdims=True) + NORM_EPS)
    return (y * gain.astype(jnp.float32)).astype(x.dtype)


def causal_dwconv3(x, w):
    s = x.shape[1]
    xp = jnp.pad(x, ((0, 0), (CONV_WIDTH - 1, 0), (0, 0)))
    return w[0] * xp[:, :s] + w[1] * xp[:, 1:s + 1] + w[2] * xp[:, 2:s + 2]


def split_heads(t, n_heads):
    b, s, _ = t.shape
    return t.reshape(b, s, n_heads, -1).transpose(0, 2, 1, 3)


def merge_heads(t):
    b, h, s, d = t.shape
    return t.transpose(0, 2, 1, 3).reshape(b, s, h * d)


def alibi_slopes(n):
    def pow2_slopes(m):
        start = 2.0 ** (-8.0 / m)
        return [start ** (i + 1) for i in range(m)]
    if math.log2(n).is_integer():
        s = pow2_slopes(n)
    else:
        c = 2 ** int(math.floor(math.log2(n)))
        s = pow2_slopes(c) + list(alibi_slopes(2 * c))[0::2][: n - c]
    return np.asarray(s, dtype=np.float32)


def chunkwise_retention(q, k, v):
    b, h, s, d = q.shape
    c = RET_CHUNK
    n = s // c
    dt = q.dtype
    lg = jnp.log1p(-jnp.exp2(-5.0 - jnp.arange(h, dtype=jnp.float32)))
    i = jnp.arange(c, dtype=jnp.float32)
    diff = i[:, None] - i[None, :]
    intra = jnp.where(diff >= 0, jnp.exp(jnp.maximum(diff, 0.0)[None] * lg[:, None, None]), 0.0)
    q_decay = jnp.exp((i + 1.0)[None] * lg[:, None])
    k_decay = jnp.exp((c - 1.0 - i)[None] * lg[:, None])
    chunk_decay = jnp.exp(c * lg).astype(dt)
    qc = q.reshape(b, h, n, c, d)
    kc = k.reshape(b, h, n, c, d)
    vc = v.reshape(b, h, n, c, d)
    scores = jnp.einsum('bhncd,bhnmd->bhncm', qc, kc) * intra[:, None].astype(dt)
    y_intra = jnp.einsum('bhncm,bhnme->bhnce', scores, vc)
    kv = jnp.einsum('bhnmd,bhnme->nbhde', kc * k_decay[:, None, :, None].astype(dt), vc)

    def step(state, kv_n):
        return chunk_decay[:, None, None] * state + kv_n, state

    _, s_prev = lax.scan(step, jnp.zeros((b, h, d, d), dt), kv)
    y_cross = jnp.einsum('bhncd,nbhde->bhnce', qc * q_decay[:, None, :, None].astype(dt), s_prev)
    return (y_intra + y_cross).reshape(b, h, s, d)


def retention_mixer(body, gn_gain):
    q, k, v, g = jnp.split(body, 4, axis=-1)
    q, k, v = (split_heads(t, RET_HEADS) for t in (q, k, v))
    y = chunkwise_retention(q, k * RET_HEAD_DIM ** -0.5, v)
    yf = y.astype(jnp.float32)
    yf = yf * lax.rsqrt(jnp.mean(yf * yf, axis=-1, keepdims=True) + NORM_EPS)
    y = (merge_heads(yf) * gn_gain.astype(jnp.float32)).astype(body.dtype)
    return jax.nn.silu(g) * y


def short_conv_mixer(body, conv_w):
    gate_b, gate_c, h = jnp.split(body, 3, axis=-1)
    return gate_b * causal_dwconv3(gate_c * h, conv_w)


def moba_attention(q, k, v, slopes):
    b, h, s, d = q.shape
    n_blk = -(-s // MOBA_BLOCK)
    s_pad = n_blk * MOBA_BLOCK
    pad = ((0, 0), (0, 0), (0, s_pad - s), (0, 0))
    kb = jnp.pad(k, pad).reshape(b, h, n_blk, MOBA_BLOCK, d)
    vb = jnp.pad(v, pad).reshape(b, h, n_blk, MOBA_BLOCK, d)
    k_mean = jnp.mean(kb, axis=3)
    t = jnp.arange(s)
    own = t // MOBA_BLOCK
    gate = jnp.einsum('bhsd,bhnd->bhsn', q, k_mean).astype(jnp.float32)
    past = jnp.arange(n_blk)[None, :] < own[:, None]
    gate = jnp.where(past, gate, -jnp.inf)
    k_eff = min(MOBA_TOPK, n_blk)
    _, top = lax.top_k(gate, k_eff)
    idx = jnp.concatenate([top, jnp.broadcast_to(own.astype(top.dtype)[None, None, :, None], (b, h, s, 1))], axis=-1)
    valid = jnp.concatenate([jnp.arange(k_eff)[None, :] < own[:, None], jnp.ones((s, 1), bool)], axis=-1)
    k1 = k_eff + 1
    n_q = s // MOBA_Q_CHUNK
    q_ch = jnp.moveaxis(q.reshape(b, h, n_q, MOBA_Q_CHUNK, d), 2, 0)
    i_ch = jnp.moveaxis(idx.reshape(b, h, n_q, MOBA_Q_CHUNK, k1), 2, 0)
    t_ch = t.reshape(n_q, MOBA_Q_CHUNK)
    v_ch = valid.reshape(n_q, MOBA_Q_CHUNK, k1)
    offs = jnp.arange(MOBA_BLOCK)
    scale = d ** -0.5
    gather = jax.vmap(jax.vmap(lambda blocks, ids: blocks[ids]))

    def attend(args):
        qc, ic, tc, vmask = args
        kg = gather(kb, ic)
        vg = gather(vb, ic)
        logits = jnp.einsum('bhqd,bhqjsd->bhqjs', qc, kg).astype(jnp.float32) * scale
        key_pos = ic[..., None] * MOBA_BLOCK + offs
        dist = (tc[:, None, None] - key_pos).astype(jnp.float32)
        logits = logits - slopes[:, None, None, None] * dist
        mask = vmask[:, :, None] & (dist >= 0)
        logits = jnp.where(mask, logits, -jnp.inf)
        shp = logits.shape
        p = jax.nn.softmax(logits.reshape(shp[0], shp[1], shp[2], -1), axis=-1).reshape(shp).astype(qc.dtype)
        return jnp.einsum('bhqjs,bhqjsd->bhqd', p, vg)

    out = lax.map(attend, (q_ch, i_ch, t_ch, v_ch))
    return jnp.moveaxis(out, 0, 2).reshape(b, h, s, d)


def moba_mixer(body):
    q, k, v = jnp.split(body, 3, axis=-1)
    q, k, v = (split_heads(t, MOBA_HEADS) for t in (q, k, v))
    slopes = jnp.asarray(alibi_slopes(MOBA_HEADS))
    return merge_heads(moba_attention(q, k, v, slopes))


def memory_attention(q_mem, mem_kv):
    q = split_heads(q_mem, MEM_HEADS)
    k, v = jnp.split(mem_kv, 2, axis=-1)
    k = split_heads(k, MEM_HEADS)
    v = split_heads(v, MEM_HEADS)
    logits = jnp.einsum('bhsd,bhmd->bhsm', q, k).astype(jnp.float32) * MEM_HEAD_DIM ** -0.5
    p = jax.nn.softmax(logits, axis=-1).astype(q.dtype)
    return merge_heads(jnp.einsum('bhsm,bhmd->bhsd', p, v))


def conv_ffn(x, w_up, conv_w, conv_b, w_down):
    h = causal_dwconv3(x @ w_up, conv_w) + conv_b
    g, u = jnp.split(h, 2, axis=-1)
    return (jax.nn.silu(g) * u) @ w_down


def setup_inputs(seed: int = 0) -> dict:
    key = jax.random.key(seed)
    keys = iter(jax.random.split(key, 64))
    f32 = jnp.float32

    def dense(fan_in, fan_out):
        return jax.random.normal(next(keys), (fan_in, fan_out), f32) * fan_in ** -0.5

    def gain(n):
        return 1.0 + 0.02 * jax.random.normal(next(keys), (n,), f32)

    inputs = {
        'x': jax.random.normal(next(keys), (BATCH, SEQ, D_MODEL), f32),
        'mem': jax.random.normal(next(keys), (BATCH, MEM_TOKENS, D_MODEL), f32),
        'mem_norm': gain(D_MODEL),
    }
    for i in range(DEPTH):
        kind = i % N_MIXERS
        p = 'l%d_' % i
        inputs[p + 'norm_mix'] = gain(D_MODEL)
        inputs[p + 'w_in'] = dense(D_MODEL, MIXER_COLS[kind] + MEM_WIDTH)
        if kind == 0:
            inputs[p + 'ret_gn'] = gain(MIX_WIDTH)
        elif kind == 1:
            inputs[p + 'conv_w'] = jax.random.normal(next(keys), (CONV_WIDTH, MIX_WIDTH), f32) * CONV_WIDTH ** -0.5
        inputs[p + 'w_mem_kv'] = dense(D_MODEL, 2 * MEM_WIDTH)
        inputs[p + 'w_o'] = dense(D_MODEL, D_MODEL)
        inputs[p + 'norm_ffn'] = gain(D_MODEL)
        inputs[p + 'ffn_w_up'] = dense(D_MODEL, 2 * D_FF)
        inputs[p + 'ffn_conv_w'] = jax.random.normal(next(keys), (CONV_WIDTH, 2 * D_FF), f32) * CONV_WIDTH ** -0.5
        inputs[p + 'ffn_conv_b'] = 0.01 * jax.random.normal(next(keys), (2 * D_FF,), f32)
        inputs[p + 'ffn_w_down'] = dense(D_FF, D_MODEL)
    inputs['final_norm'] = gain(D_MODEL)
    return inputs


def reference(x, mem, mem_norm,
              l0_norm_mix, l0_w_in, l0_ret_gn, l0_w_mem_kv, l0_w_o, l0_norm_ffn, l0_ffn_w_up, l0_ffn_conv_w, l0_ffn_conv_b, l0_ffn_w_down,
              l1_norm_mix, l1_w_in, l1_conv_w, l1_w_mem_kv, l1_w_o, l1_norm_ffn, l1_ffn_w_up, l1_ffn_conv_w, l1_ffn_conv_b, l1_ffn_w_down,
              l2_norm_mix, l2_w_in, l2_w_mem_kv, l2_w_o, l2_norm_ffn, l2_ffn_w_up, l2_ffn_conv_w, l2_ffn_conv_b, l2_ffn_w_down,
              l3_norm_mix, l3_w_in, l3_ret_gn, l3_w_mem_kv, l3_w_o, l3_norm_ffn, l3_ffn_w_up, l3_ffn_conv_w, l3_ffn_conv_b, l3_ffn_w_down,
              final_norm):
    layers = [
        (l0_norm_mix, l0_w_in, l0_ret_gn, l0_w_mem_kv, l0_w_o, l0_norm_ffn, l0_ffn_w_up, l0_ffn_conv_w, l0_ffn_conv_b, l0_ffn_w_down),
        (l1_norm_mix, l1_w_in, l1_conv_w, l1_w_mem_kv, l1_w_o, l1_norm_ffn, l1_ffn_w_up, l1_ffn_conv_w, l1_ffn_conv_b, l1_ffn_w_down),
        (l2_norm_mix, l2_w_in, None, l2_w_mem_kv, l2_w_o, l2_norm_ffn, l2_ffn_w_up, l2_ffn_conv_w, l2_ffn_conv_b, l2_ffn_w_down),
        (l3_norm_mix, l3_w_in, l3_ret_gn, l3_w_mem_kv, l3_w_o, l3_norm_ffn, l3_ffn_w_up, l3_ffn_conv_w, l3_ffn_conv_b, l3_ffn_w_down),
    ]
    mem_n = rms_norm(mem, mem_norm)
    for i in range(DEPTH):
        norm_mix, w_in, extra, w_mem_kv, w_o, norm_ffn, w_up, cw, cb, w_down = layers[i]
        kind = i % N_MIXERS
        h = rms_norm(x, norm_mix)
        proj = h @ w_in
        body, q_mem = proj[..., :-MEM_WIDTH], proj[..., -MEM_WIDTH:]
        if kind == 0:
            tok = retention_mixer(body, extra)
        elif kind == 1:
            tok = short_conv_mixer(body, extra)
        else:
            tok = moba_mixer(body)
        mem_out = memory_attention(q_mem, mem_n @ w_mem_kv)
        x = x + jnp.concatenate([tok, mem_out], axis=-1) @ w_o
        x = x + conv_ffn(rms_norm(x, norm_ffn), w_up, cw, cb, w_down)
    return rms_norm(x, final_norm)
```

```python
from contextlib import ExitStack
import math
import numpy as np
import ml_dtypes
import concourse.bass as bass
import concourse.mybir as mybir
from concourse.bass_utils import run_bass_kernel_spmd

F32 = mybir.dt.float32
BF16 = mybir.dt.bfloat16
AF = mybir.ActivationFunctionType
ALU = mybir.AluOpType
AX = mybir.AxisListType

D = 2048
NK = 16
FF = 5632
NF = 44
EPS = 1e-6
MEM = 256
TW = 512
KINDS = (0, 1, 2, 0)
NCOLS = (6656, 5120, 5120, 6656)

ENGS = ["pe", "act", "dve", "pool", "sp"]
N_DMA_SEMS = 16


class Tok:
    __slots__ = ("last_w", "readers", "excl")

    def __init__(self, excl=False):
        self.last_w = None
        self.readers = []
        self.excl = excl


class Op:
    __slots__ = ("eng", "pos", "fn", "waits", "is_dma", "dma_sem", "dma_val", "waited", "sig")

    def __init__(self, eng, pos, fn, is_dma):
        self.eng = eng
        self.pos = pos
        self.fn = fn
        self.waits = []
        self.is_dma = is_dma
        self.dma_sem = None
        self.dma_val = None
        self.waited = False
        self.sig = None


class Prog:
    def __init__(self, nc):
        self.nc = nc
        self.streams = {e: [] for e in ENGS}
        self.seen = {e: {} for e in ENGS}
        self.dma_count = {e: 0 for e in ENGS}
        self.dma_ops = {e: [] for e in ENGS}
        self.pending = {e: [] for e in ENGS}

    def op(self, eng, fn, reads=(), writes=(), dma=False):
        st = self.streams[eng]
        o = Op(eng, len(st), fn, dma)
        if any(t.excl for t in reads):
            writes = list(writes) + [t for t in reads if t.excl]
            reads = [t for t in reads if not t.excl]
        deps = self.pending[eng]
        self.pending[eng] = []
        if dma:
            k = self.dma_count[eng]
            self.dma_count[eng] = k + 1
            o.dma_sem = k % N_DMA_SEMS
            o.dma_val = 16 * (k // N_DMA_SEMS + 1)
            self.dma_ops[eng].append(o)
            if k >= N_DMA_SEMS:
                deps.append(self.dma_ops[eng][k - N_DMA_SEMS])
        for t in reads:
            if t.last_w is not None:
                deps.append(t.last_w)
        for t in writes:
            if t.last_w is not None:
                deps.append(t.last_w)
            deps.extend(t.readers)
        seen = self.seen[eng]
        for d in deps:
            if d is o:
                continue
            if d.is_dma:
                key = ("dma", d.eng, d.dma_sem)
                val = d.dma_val
            else:
                if d.eng == eng and eng == "pe":
                    continue
                key = ("eng", d.eng)
                val = d.pos
            if seen.get(key, -1) >= val:
                continue
            seen[key] = val
            o.waits.append(d)
            d.waited = True
        for t in reads:
            t.readers.append(o)
        for t in writes:
            t.last_w = o
            t.readers = []
        st.append(o)
        return o

    def barrier(self):
        lasts = []
        for e in ("pe", "act", "dve", "pool"):
            for o in reversed(self.streams[e]):
                if not o.is_dma:
                    lasts.append(o)
                    break
            lasts.extend(self.dma_ops[e][-N_DMA_SEMS:])
        for e in ("pe", "act", "dve", "pool"):
            self.pending[e] = list(lasts)

    def emit(self):
        nc = self.nc
        with ExitStack() as es:
            esem = {e: es.enter_context(nc.semaphore("s_" + e)) for e in ENGS}
            dsem = {e: [es.enter_context(nc.semaphore("d_%s%d" % (e, i))) for i in range(N_DMA_SEMS)]
                    for e in ENGS if self.dma_count[e] > 0}
            for e in ENGS:
                c = 0
                for o in self.streams[e]:
                    if (not o.is_dma) and o.waited:
                        c += 1
                        o.sig = c
            block = es.enter_context(nc.Block())

            def run(engname, h):
                for o in self.streams[engname]:
                    for d in o.waits:
                        if d.is_dma:
                            h.wait_ge(dsem[d.eng][d.dma_sem], d.dma_val)
                        else:
                            h.wait_ge(esem[d.eng], d.sig)
                    ins = o.fn(h)
                    if o.is_dma:
                        ins.then_inc(dsem[engname][o.dma_sem], 16)
                    elif o.waited:
                        ins.then_inc(esem[engname], 1)
                if engname == "sp":
                    for e2 in dsem:
                        n = self.dma_count[e2]
                        for i in range(N_DMA_SEMS):
                            cnt = (n - i + N_DMA_SEMS - 1) // N_DMA_SEMS if n > i else 0
                            if cnt > 0:
                                h.wait_ge(dsem[e2][i], 16 * cnt)

            @block.tensor
            def _(h):
                run("pe", h)

            @block.scalar
            def _(h):
                run("act", h)

            @block.vector
            def _(h):
                run("dve", h)

            @block.gpsimd
            def _(h):
                run("pool", h)

            @block.sync
            def _(h):
                run("sp", h)


class Ring:
    def __init__(self, bufs, excl=False):
        self.bufs = bufs
        self.toks = [Tok(excl) for _ in bufs]
        self.i = 0

    def get(self):
        j = self.i % len(self.bufs)
        self.i += 1
        return self.bufs[j], self.toks[j]


def alibi_slopes(n):
    def pow2(m):
        start = 2.0 ** (-8.0 / m)
        return [start ** (i + 1) for i in range(m)]
    if math.log2(n).is_integer():
        return pow2(n)
    c = 2 ** int(math.floor(math.log2(n)))
    return pow2(c) + list(alibi_slopes(2 * c))[0::2][: n - c]


def make_consts():
    c = {}
    c["ident"] = np.eye(128, dtype=np.float32)
    lg = np.log1p(-np.exp2(-5.0 - np.arange(6, dtype=np.float64)))
    i = np.arange(128, dtype=np.float64)
    diff = i[:, None] - i[None, :]
    intra = np.where(diff >= 0, np.exp(np.maximum(diff, 0.0)[None] * lg[:, None, None]), 0.0)
    c["intraT"] = np.ascontiguousarray((intra / 16.0).transpose(2, 0, 1)).astype(np.float32)
    qd = np.exp((i + 1.0)[None] * lg[:, None])
    c["qdec"] = np.ascontiguousarray(np.broadcast_to(qd[None], (128, 6, 128))).astype(np.float32)
    kd = np.exp((127.0 - i)[None] * lg[:, None]) / 16.0
    c["kdec"] = np.ascontiguousarray(kd.T).astype(np.float32)
    c["cdec"] = [float(np.exp(128.0 * v)) for v in lg]
    sl = np.asarray(alibi_slopes(12), dtype=np.float64)
    p = np.arange(128, dtype=np.float64)
    dl = np.arange(32, dtype=np.float64)
    c["btab"] = (-sl[None, :, None] * (128.0 * dl[None, None, :] + 63.5 - p[:, None, None])).astype(np.float32)
    c["causal"] = (p[:, None] <= p[None, :]).astype(np.float32)
    own = np.arange(16)[:, None]
    n = np.arange(16)[None, :]
    gm = np.where(n < own, 0.0, -1e30).astype(np.float32)
    va = (n < own).astype(np.float32)
    oh = (n == own).astype(np.float32)
    c["gmask"] = np.ascontiguousarray(np.broadcast_to(gm[None], (128, 16, 16)))
    c["gvalid"] = np.ascontiguousarray(np.broadcast_to(va[None], (128, 16, 16)))
    c["gown"] = np.ascontiguousarray(np.broadcast_to(oh[None], (128, 16, 16)))
    return c


CONST_SHAPES = {"ident": [128, 128], "intraT": [128, 6, 128], "qdec": [128, 6, 128], "kdec": [128, 6],
                "btab": [128, 12, 32], "causal": [128, 128], "gmask": [128, 16, 16], "gvalid": [128, 16, 16],
                "gown": [128, 16, 16]}


def vec16(v):
    return np.ascontiguousarray(np.asarray(v, np.float32).reshape(16, 128).T)


def wchunks(w):
    w = np.asarray(w, np.float32)
    K, N = w.shape
    return np.ascontiguousarray(w.reshape(K // 128, 128, N // 128, 128).transpose(2, 1, 0, 3))


class _Stop(Exception):
    pass


def build(T, kinds=KINDS, do_final=True, stop=0):
    nc = bass.Bass("TRN2", target_bir_lowering=False)
    L = len(kinds)
    NW = T // TW
    NT = T // 128
    P = Prog(nc)
    es = ExitStack()

    def din(name, shape, dt=F32):
        return nc.dram_tensor(name, list(shape), dt, kind="ExternalInput").ap()

    xT_in = din("xT", [D, T])
    memT_in = din("memT", [D, MEM])
    gains_in = din("gains", [128, 2 * L + 2, 16])
    cst_in = {k: din("c_" + k, s) for k, s in CONST_SHAPES.items()}
    lw = []
    for li, kind in enumerate(kinds):
        nch = NCOLS[kind if kind < 3 else 0] // 128
        d = {"w_in": din("l%d_w_in" % li, [nch, 128, 16, 128]),
             "w_kv": din("l%d_w_kv" % li, [8, 128, 16, 128]),
             "w_o": din("l%d_w_o" % li, [16, 128, 16, 128]),
             "w_up": din("l%d_w_up" % li, [88, 128, 16, 128]),
             "w_dn": din("l%d_w_dn" % li, [16, 4, 128, 11, 128]),
             "cw": din("l%d_cw" % li, [128, 3, 88]),
             "cb": din("l%d_cb" % li, [128, 88])}
        if kind == 0:
            d["gn"] = din("l%d_gn" % li, [128, 12])
        if kind == 1:
            d["scw"] = din("l%d_scw" % li, [128, 3, 12])
        lw.append(d)
    out_d = nc.dram_tensor("outT", [D, T], F32, kind="ExternalOutput").ap()
    xs_d = nc.dram_tensor("xscr", [D, T], F32, kind="Internal").ap()
    kh_d = nc.dram_tensor("khist", [12, 128, T], BF16, kind="Internal").ap()
    vh_d = nc.dram_tensor("vhist", [12, 128, NT, 130], BF16, kind="Internal").ap()
    NCHMAX = 228
    wsc_d = nc.dram_tensor("wsc", [2, NCHMAX, 128, 16, 128], BF16, kind="Internal").ap()
    wsc_tok = [[Tok() for _ in range(NCHMAX)] for _ in range(2)]
    chunks = []
    widx = []
    for li_, kind_ in enumerate(kinds):
        W_ = lw[li_]
        cl = []
        for j in range(8):
            cl.append((("kv", j), W_["w_kv"][j], 16))
        for j in range(NCOLS[kind_] // 128):
            cl.append((("in", j), W_["w_in"][j], 16))
        for j in range(16):
            cl.append((("o", j), W_["w_o"][j], 16))
        for j in range(88):
            cl.append((("up", j), W_["w_up"][j], 16))
        for j in range(16):
            for p_ in range(4):
                cl.append((("dn", j, p_), W_["w_dn"][j][p_], 11))
        chunks.append(cl)
        widx.append({k: (i, kc) for i, (k, _, kc) in enumerate(cl)})
    kh_tok = [Tok() for _ in range(12)]
    vh_tok = [Tok() for _ in range(12)]
    xs_tok = Tok()

    def sb(name, shape, dt):
        return es.enter_context(nc.sbuf_tensor("sb_" + name, list(shape), dt))

    def ps(name, dt=F32, n=512):
        return es.enter_context(nc.psum_tensor(name, [128, n], dt))

    ones_f = sb("ones_f", [128, 128], F32)
    ones_b = sb("ones_b", [128, 128], BF16)
    gains = sb("gains", [128, 2 * L + 2, 16], F32)
    cst = {k: sb("c_" + k, s, F32) for k, s in CONST_SHAPES.items()}
    causal_b = sb("causal_b", [128, 128], BF16)
    cw_sb = sb("cw", [128, 3, 88], F32)
    cb_sb = sb("cb", [128, 88], F32)
    gn_sb = sb("gn", [128, 12], F32)
    scw_sb = sb("scw", [128, 3, 12], F32)
    small_tok = Tok()
    x_sb = sb("x_sb", [128, NK, TW], F32)
    x_tok = [Tok() for _ in range(NK)]
    hT = sb("hT", [128, NK, TW], BF16)
    h_tok = [Tok() for _ in range(NK)]
    rstd = sb("rstd", [128, TW], F32)
    rstd_tok = Tok()
    sq_ring = Ring([sb("sq%d" % i, [128, TW], F32) for i in range(2)])
    stg_ring = Ring([sb("stg%d" % i, [128, 16, 128], F32) for i in range(2)])
    wbf_ring = Ring([sb("wbf%d" % i, [128, 16, 128], BF16) for i in range(4)])
    halo_f = sb("halo_f", [128, 88, 2], F32)
    halo_tok = [Tok() for _ in range(88)]
    halo_s = sb("halo_s", [128, 12, 2], F32)
    halos_tok = [Tok() for _ in range(12)]
    mem_nT = sb("mem_nT", [128, NK, MEM], BF16)
    memn_tok = Tok()
    memk = sb("memk", [128, 4, MEM], BF16)
    memv = sb("memv", [128, 2, 512], BF16)
    memkv_tok = Tok()
    kmean = sb("kmean", [128, 12, 16], BF16)
    kmean_tok = Tok()
    ABF = 63 * TW
    AF32 = 14 * TW
    arena_b = sb("arena_b", [128, ABF], BF16)
    arena_f = sb("arena_f", [128, AF32], F32)

    class Carver:
        def __init__(self, t, n):
            self.t, self.n, self.o = t, n, 0

        def take(self, shape):
            sz = int(np.prod(shape))
            assert self.o + sz <= self.n, (self.o, sz, self.n)
            v = self.t[:, self.o:self.o + sz]
            self.o += sz
            if len(shape) == 2:
                v = v.rearrange("p (a b) -> p a b", a=shape[0])
            elif len(shape) == 3:
                v = v.rearrange("p (a b c) -> p a b c", a=shape[0], b=shape[1])
            return v

    psA = Ring([ps("psA%d" % i) for i in range(4)], excl=True)
    psB = Ring([ps("psB%d" % i) for i in range(2)], excl=True)
    psC = Ring([ps("psC%d" % i) for i in range(2)], excl=True)

    def mm(out, lhsT, rhs, start, stop, reads, writes):
        P.op("pe", lambda h: h.matmul(out, lhsT, rhs, start=start, stop=stop), reads, writes)

    def transpose(out, in_, ident, reads, writes):
        P.op("pe", lambda h: h.transpose(out, in_, ident), reads, writes)

    def act(out, in_, func, reads, writes, scale=None, bias=None):
        kw = {}
        if scale is not None:
            kw["scale"] = scale
        if bias is not None:
            kw["bias"] = bias
        P.op("act", lambda h: h.activation(out, in_, func, **kw), reads, writes)

    def tt(eng, out, a, b, op, reads, writes):
        P.op(eng, lambda h: h.tensor_tensor(out, a, b, op), reads, writes)

    def ts(eng, out, a, s1, s2, op0, op1, reads, writes):
        if op1 is None:
            P.op(eng, lambda h: h.tensor_scalar(out, a, s1, None, op0), reads, writes)
        else:
            P.op(eng, lambda h: h.tensor_scalar(out, a, s1, s2, op0, op1), reads, writes)

    def stt(out, a, s, b, op0, op1, reads, writes):
        P.op("dve", lambda h: h.scalar_tensor_tensor(out, a, s, b, op0, op1), reads, writes)

    def cp(eng, out, in_, reads, writes):
        if eng == "act":
            P.op("act", lambda h: h.activation(out, in_, AF.Copy), reads, writes)
        else:
            P.op(eng, lambda h: h.tensor_copy(out, in_), reads, writes)

    def dma(q, out, in_, reads, writes):
        P.op(q, lambda h: h.dma_start(out=out, in_=in_), reads, writes, dma=True)

    cur = {"li": 0, "w": 0, "pc0": 0, "nc": 0}

    def precast(li_, i0, i1):
        buf = li_ % 2
        for i in range(i0, i1):
            _, src, kc = chunks[li_][i]
            dma("pool", wsc_d[buf][i][:, 0:kc, :], src, [], [wsc_tok[buf][i]])

    def load_w(key):
        li_ = cur["li"]
        i, kc = widx[li_][key]
        wb, wtok = wbf_ring.get()
        if li_ == 0 and cur["w"] == 0:
            _, src, _ = chunks[0][i]
            stg, stok = stg_ring.get()
            dma("sp", stg[:, 0:kc, :], src, [], [stok])
            ceng = ("pool", "dve", "act")[cur["nc"] % 3]
            cur["nc"] += 1
            cp(ceng, wb[:, 0:kc, :], stg[:, 0:kc, :], [stok], [wtok])
            n0 = cur["pc0"]
            n1 = min(n0 + 2, len(chunks[0]))
            precast(0, n0, n1)
            cur["pc0"] = n1
        else:
            assert wsc_tok[li_ % 2][i].last_w is not None, (li_, key, i)
            dma("sp", wb[:, 0:kc, :], wsc_d[li_ % 2][i][:, 0:kc, :], [wsc_tok[li_ % 2][i]], [wtok])
        return wb, wtok

    c_tok = Tok()
    P.op("pool", lambda h: h.memset(ones_f[:], 1.0), [], [c_tok])
    P.op("pool", lambda h: h.memset(ones_b[:], 1.0), [], [c_tok])
    P.op("pool", lambda h: h.memset(kmean[:], 0.0), [], [kmean_tok])
    P.op("pool", lambda h: h.memset(halo_f[:], 0.0), [], halo_tok)
    P.op("pool", lambda h: h.memset(halo_s[:], 0.0), [], halos_tok)
    dma("sp", gains[:], gains_in, [], [c_tok])
    for k in CONST_SHAPES:
        dma("sp", cst[k][:], cst_in[k], [], [c_tok])
    cp("pool", causal_b[:], cst["causal"][:], [c_tok], [c_tok])

    def rmsnorm_to_h(gidx, n, src=None, src_toks=None, dst=None, dst_toks=None):
        src = x_sb if src is None else src
        src_toks = x_tok if src_toks is None else src_toks
        dst = hT if dst is None else dst
        dst_toks = h_tok if dst_toks is None else dst_toks
        pst, ptok = psC.get()
        for c in range(NK):
            sq, sqt = sq_ring.get()
            act(sq[:, 0:n], src[:, c, 0:n], AF.Square, [src_toks[c]], [sqt])
            mm(pst[:, 0:n], ones_f[:], sq[:, 0:n], c == 0, c == NK - 1, [sqt, c_tok], [ptok])
        ts("dve", rstd[:, 0:n], pst[:, 0:n], 1.0 / D, EPS, ALU.mult, ALU.add, [ptok], [rstd_tok])
        act(rstd[:, 0:n], rstd[:, 0:n], AF.Sqrt, [rstd_tok], [rstd_tok])
        P.op("dve", lambda h: h.reciprocal(rstd[:, 0:n], rstd[:, 0:n]), [rstd_tok], [rstd_tok])
        for c in range(NK):
            stt(dst[:, c, 0:n], src[:, c, 0:n], gains[:, gidx, c:c + 1], rstd[:, 0:n], ALU.mult, ALU.mult,
                [src_toks[c], rstd_tok, c_tok], [dst_toks[c]])

    dma("sp", x_sb[:, :, 0:MEM], memT_in.rearrange("(c p) t -> p c t", p=128), [], x_tok)
    rmsnorm_to_h(0, MEM, dst=mem_nT, dst_toks=[memn_tok] * NK)

    def proj_fm(wb, wtok, n=TW, rhs=None, rhs_toks=None):
        rhs = hT if rhs is None else rhs
        rhs_toks = h_tok if rhs_toks is None else rhs_toks
        pt, ptok = psA.get()
        for k in range(NK):
            mm(pt[:, 0:n], wb[:, k, :], rhs[:, k, 0:n], k == 0, k == NK - 1, [wtok, rhs_toks[k]], [ptok])
        return pt, ptok

    def proj_tm(pt, ptok, col0, wb, wtok, tile, lhs=None, lhs_toks=None):
        lhs = hT if lhs is None else lhs
        lhs_toks = h_tok if lhs_toks is None else lhs_toks
        for k in range(NK):
            mm(pt[:, col0:col0 + 128], lhs[:, k, tile * 128:(tile + 1) * 128], wb[:, k, :], k == 0, k == NK - 1,
               [wtok, lhs_toks[k]], [ptok])

    def chk(n):
        if stop == n:
            dma("act", out_d[:, 0:TW].rearrange("(c p) t -> p c t", p=128), x_sb[:], x_tok, [xs_tok])
            raise _Stop()

    try:
        for li, kind in enumerate(kinds):
            W = lw[li]
            src_d = xT_in if li == 0 else xs_d
            last = (li == L - 1)
            chk(1)
            cur["li"] = li
            cur["w"] = 0
            P.barrier()
            dma("sp", cw_sb[:], W["cw"], [], [small_tok])
            dma("sp", cb_sb[:], W["cb"], [], [small_tok])
            if kind == 0:
                dma("sp", gn_sb[:], W["gn"], [], [small_tok])
            if kind == 1:
                dma("sp", scw_sb[:], W["scw"], [], [small_tok])
            P.op("pool", lambda h: h.memset(halo_f[:], 0.0), [], halo_tok)
            for j in range(4):
                wb, wtok = load_w(("kv", j))
                pt, ptok = proj_fm(wb, wtok, n=MEM, rhs=mem_nT, rhs_toks=[memn_tok] * NK)
                cp("act", memk[:, j, :], pt[:, 0:MEM], [ptok], [memkv_tok])
            for mt in range(2):
                pt, ptok = psA.get()
                for j in range(4):
                    wb, wtok = load_w(("kv", 4 + j))
                    proj_tm(pt, ptok, j * 128, wb, wtok, mt, lhs=mem_nT, lhs_toks=[memn_tok] * NK)
                cp("act", memv[:, mt, :], pt[:, :], [ptok], [memkv_tok])

            P.barrier()
            P.op("pool", lambda h: h.memset(arena_b[:], 0.0), [], [])
            P.op("pool", lambda h: h.memset(arena_f[:], 0.0), [], [])
            P.barrier()
            cb_ = Carver(arena_b, ABF)
            cf_ = Carver(arena_f, AF32)
            catT = cb_.take([NK, TW])
            cat_tok = [Tok() for _ in range(NK)]
            qm_sb = cb_.take([TW])
            qm_tok = Tok()
            pm_sb = [cb_.take([TW]) for _ in range(2)]
            pm_tok = [Tok(), Tok()]
            rden = cf_.take([TW])
            rden_tok = Tok()
            if kind == 0:
                qT_sb = cb_.take([2, TW]); qTd_sb = cb_.take([2, TW]); kT_sb = cb_.take([2, TW]); sg_sb = cb_.take([2, TW])
                q_tok, qd_tok, k_tok, sg_tok = Tok(), Tok(), Tok(), Tok()
                ktok_sb = cb_.take([4, 256]); vtok_sb = cb_.take([4, 256])
                kt_tok = [Tok() for _ in range(4)]; vt_tok = [Tok() for _ in range(4)]
                S_b = cb_.take([6, 2, 256]); Sb_tok = [Tok() for _ in range(6)]
                sT_sb = [cb_.take([128]) for _ in range(2)]; sT_ring = Ring(sT_sb)
                S_f = cf_.take([6, 2, 256]); Sf_tok = [Tok() for _ in range(6)]
                y_sb = cf_.take([2, TW]); y_tok = Tok()
            if kind == 1:
                c_sb = cf_.take([TW]); cs_tok = Tok()
                ext_s = cf_.take([TW + 2]); exts_tok = Tok()
                t_s = cf_.take([TW]); ts_tok = Tok()
                P.op("pool", lambda h: h.memset(halo_s[:], 0.0), [], halos_tok)
            if kind == 2:
                qT_l = [cb_.take([TW]) for _ in range(2)]; q_tl = [Tok(), Tok()]
                kT_l = [cb_.take([TW]) for _ in range(2)]; k_tl = [Tok(), Tok()]
                va_l = [cb_.take([4, 130]) for _ in range(2)]; va_tl = [Tok(), Tok()]
                kTh_l = [cb_.take([T]) for _ in range(2)]; kTh_tl = [Tok(), Tok()]
                vh = cb_.take([NT, 130]); vh_tok_sb = Tok()
                pT_ring = Ring([cb_.take([128]) for _ in range(5)])
                acc = cf_.take([4, 132]); acc_tok = [Tok() for _ in range(4)]
                g_sb = cf_.take([16]); g_tok = Tok()
                top8 = cf_.take([8]); top_tok = Tok()
                sel = cf_.take([4, 16]); sel_tok = [Tok() for _ in range(4)]
                rec = cf_.take([4]); rec_tok = Tok()
                o_sb = cf_.take([128]); o_tok = Tok()
                kred = cf_.take([2]); kred_tok = Tok()
                P.op("pool", lambda h: h.memset(kmean[:], 0.0), [], [kmean_tok])
                for va_sb, va_tok in zip(va_l, va_tl):
                    P.op("pool", lambda h, va_sb=va_sb: h.memset(va_sb, 1.0), [], [va_tok])
            fb = dict(actT=cb_.take([11, TW]), a_tok=[Tok() for _ in range(11)],
                      ext=[cf_.take([TW + 2]) for _ in range(2)], ext_tok=[Tok(), Tok()],
                      tg=cf_.take([TW]), tg_tok=Tok(), tu=cf_.take([TW]), tu_tok=Tok())

            for w in range(NW):
                t0 = w * TW
                chk(2)
                cur["w"] = w
                if li == 0 and w == 1:
                    precast(0, cur["pc0"], len(chunks[0]))
                    cur["pc0"] = len(chunks[0])
                if li + 1 < L:
                    nchn = len(chunks[li + 1])
                    precast(li + 1, (w * nchn) // NW, ((w + 1) * nchn) // NW)
                dma("sp", x_sb[:], src_d[:, t0:t0 + TW].rearrange("(c p) t -> p c t", p=128), [xs_tok], x_tok)
                rmsnorm_to_h(1 + 2 * li, TW)

                chk(3)
                if kind == 0:
                    for hd in range(6):
                        for dk in range(2):
                            wb, wtok = load_w(("in", 2 * hd + dk))
                            pt, ptok = proj_fm(wb, wtok)
                            cp("act", qT_sb[:, dk, :], pt[:, :], [ptok], [q_tok])
                            for n4 in range(4):
                                tt("dve", qTd_sb[:, dk, n4 * 128:(n4 + 1) * 128], pt[:, n4 * 128:(n4 + 1) * 128],
                                   cst["qdec"][:, hd, :], ALU.mult, [ptok, c_tok], [qd_tok])
                        ptk = [psB.get() for _ in range(2)]
                        for dk in range(2):
                            wb, wtok = load_w(("in", 12 + 2 * hd + dk))
                            pt, ptok = proj_fm(wb, wtok)
                            cp("act", kT_sb[:, dk, :], pt[:, :], [ptok], [k_tok])
                            for tl in range(4):
                                proj_tm(ptk[tl // 2][0], ptk[tl // 2][1], (tl % 2) * 256 + dk * 128, wb, wtok, tl)
                        for tl in range(4):
                            ts("dve", ktok_sb[:, tl, :], ptk[tl // 2][0][:, (tl % 2) * 256:(tl % 2) * 256 + 256],
                               cst["kdec"][:, hd:hd + 1], None, ALU.mult, None, [ptk[tl // 2][1], c_tok], [kt_tok[tl]])
                        ptv = [psB.get() for _ in range(2)]
                        for dk in range(2):
                            wb, wtok = load_w(("in", 24 + 2 * hd + dk))
                            for tl in range(4):
                                proj_tm(ptv[tl // 2][0], ptv[tl // 2][1], (tl % 2) * 256 + dk * 128, wb, wtok, tl)
                        for tl in range(4):
                            cp("act", vtok_sb[:, tl, :], ptv[tl // 2][0][:, (tl % 2) * 256:(tl % 2) * 256 + 256],
                               [ptv[tl // 2][1]], [vt_tok[tl]])
                        for dk in range(2):
                            wb, wtok = load_w(("in", 36 + 2 * hd + dk))
                            pt, ptok = proj_fm(wb, wtok)
                            act(sg_sb[:, dk, :], pt[:, :], AF.Silu, [ptok], [sg_tok])
                        py = [psB.get() for _ in range(2)]
                        for n in range(4):
                            cs = slice(n * 128, (n + 1) * 128)
                            pst, pstok = psC.get()
                            for dk in range(2):
                                mm(pst[:, 0:128], kT_sb[:, dk, cs], qT_sb[:, dk, cs], dk == 0, dk == 1, [k_tok, q_tok], [pstok])
                            sT, sTtok = sT_ring.get()
                            tt("dve", sT, pst[:, 0:128], cst["intraT"][:, hd, :], ALU.mult, [pstok, c_tok], [sTtok])
                            for ec in range(2):
                                mm(py[ec][0][:, cs], vtok_sb[:, n, ec * 128:(ec + 1) * 128], sT, True, False,
                                   [vt_tok[n], sTtok], [py[ec][1]])
                                for dk in range(2):
                                    mm(py[ec][0][:, cs], S_b[:, hd, dk, ec * 128:(ec + 1) * 128], qTd_sb[:, dk, cs], False, dk == 1,
                                       [Sb_tok[hd], qd_tok], [py[ec][1]])
                            pkv, pkvtok = psC.get()
                            for dk in range(2):
                                mm(pkv[:, dk * 256:(dk + 1) * 256], ktok_sb[:, n, dk * 128:(dk + 1) * 128], vtok_sb[:, n, :], True, True,
                                   [kt_tok[n], vt_tok[n]], [pkvtok])
                            Sf = S_f[:, hd, :, :].rearrange("p a b -> p (a b)")
                            stt(Sf, Sf, cst_cdec[hd], pkv[:, :], ALU.mult, ALU.add, [pkvtok, Sf_tok[hd]], [Sf_tok[hd]])
                            cp("pool", S_b[:, hd, :, :].rearrange("p a b -> p (a b)"), Sf, [Sf_tok[hd]], [Sb_tok[hd]])
                        pst, pstok = psC.get()
                        for ec in range(2):
                            cp("act", y_sb[:, ec, :], py[ec][0][:, :], [py[ec][1]], [y_tok])
                            sq, sqt = sq_ring.get()
                            act(sq[:], py[ec][0][:, :], AF.Square, [py[ec][1]], [sqt])
                            mm(pst[:, :], ones_f[:], sq[:], ec == 0, ec == 1, [sqt, c_tok], [pstok])
                        ts("dve", rden, pst[:, :], 1.0 / 256, EPS, ALU.mult, ALU.add, [pstok], [rden_tok])
                        act(rden, rden, AF.Sqrt, [rden_tok], [rden_tok])
                        P.op("dve", lambda h, rden=rden: h.reciprocal(rden, rden), [rden_tok], [rden_tok])
                        for ec in range(2):
                            stt(y_sb[:, ec, :], y_sb[:, ec, :], gn_sb[:, 2 * hd + ec:2 * hd + ec + 1], rden, ALU.mult, ALU.mult,
                                [y_tok, rden_tok, small_tok], [y_tok])
                            tt("dve", catT[:, 2 * hd + ec, :], y_sb[:, ec, :], sg_sb[:, ec, :], ALU.mult, [y_tok, sg_tok],
                               [cat_tok[2 * hd + ec]])
                    qm_base = 48
                elif kind == 1:
                    for j in range(12):
                        wb, wtok = load_w(("in", 12 + j))
                        pc, pctok = proj_fm(wb, wtok)
                        cp("act", c_sb, pc[:, :], [pctok], [cs_tok])
                        wb, wtok = load_w(("in", 24 + j))
                        ph, phtok = proj_fm(wb, wtok)
                        cp("pool", ext_s[:, 0:2], halo_s[:, j, :], [halos_tok[j]], [exts_tok])
                        tt("dve", ext_s[:, 2:TW + 2], ph[:, :], c_sb, ALU.mult, [phtok, cs_tok], [exts_tok])
                        cp("pool", halo_s[:, j, :], ext_s[:, TW:TW + 2], [exts_tok], [halos_tok[j]])
                        act(t_s, ext_s[:, 2:TW + 2], AF.Identity, [exts_tok, small_tok], [ts_tok], scale=scw_sb[:, 2, j:j + 1])
                        stt(t_s, ext_s[:, 1:TW + 1], scw_sb[:, 1, j:j + 1], t_s, ALU.mult, ALU.add, [exts_tok, ts_tok], [ts_tok])
                        stt(t_s, ext_s[:, 0:TW], scw_sb[:, 0, j:j + 1], t_s, ALU.mult, ALU.add, [exts_tok, ts_tok], [ts_tok])
                        wb, wtok = load_w(("in", j))
                        pb, pbtok = proj_fm(wb, wtok)
                        tt("dve", catT[:, j, :], pb[:, :], t_s, ALU.mult, [pbtok, ts_tok], [cat_tok[j]])
                    qm_base = 36
                else:
                    for hd in range(12):
                        qT_sb, q_tok = qT_l[hd % 2], q_tl[hd % 2]
                        kT_sb, k_tok = kT_l[hd % 2], k_tl[hd % 2]
                        va_sb, va_tok = va_l[hd % 2], va_tl[hd % 2]
                        kTh, kTh_tok = kTh_l[hd % 2], kTh_tl[hd % 2]
                        wb, wtok = load_w(("in", hd))
                        pt, ptok = proj_fm(wb, wtok)
                        cp("act", qT_sb, pt[:, :], [ptok], [q_tok])
                        wb, wtok = load_w(("in", 12 + hd))
                        pt, ptok = proj_fm(wb, wtok)
                        cp("act", kT_sb, pt[:, :], [ptok], [k_tok])
                        P.op("dve", lambda h, kT_sb=kT_sb, kred=kred: h.tensor_reduce(
                            kred, kT_sb.rearrange("p (a b) -> p a b", a=2), AX.X, ALU.add), [k_tok], [kred_tok])
                        cp("pool", kmean[:, hd, 2 * w:2 * w + 2], kred, [kred_tok], [kmean_tok])
                        dma("act", kh_d[hd][:, t0:t0 + TW], kT_sb, [k_tok], [kh_tok[hd]])
                        wb, wtok = load_w(("in", 24 + hd))
                        pv, pvtok = psA.get()
                        for tl in range(4):
                            proj_tm(pv, pvtok, tl * 128, wb, wtok, tl)
                        cp("act", va_sb[:, :, 0:128], pv[:, :].rearrange("p (a b) -> p a b", a=4), [pvtok], [va_tok])
                        dma("act", vh_d[hd][:, 4 * w:4 * w + 4, :], va_sb, [va_tok], [vh_tok[hd]])
                        nkt = 4 * (w + 1)
                        dma("act", kTh[:, 0:t0 + TW], kh_d[hd][:, 0:t0 + TW], [kh_tok[hd]], [kTh_tok])
                        dma("act", vh[:, 0:nkt, :], vh_d[hd][:, 0:nkt, :], [vh_tok[hd]], [vh_tok_sb])
                        jobs = []
                        for i in range(4):
                            qi = 4 * w + i
                            own = qi // 2
                            qs = slice(i * 128, (i + 1) * 128)
                            pg, pgtok = psC.get()
                            mm(pg[:, 0:16], qT_sb[:, qs], kmean[:, hd, :], True, True, [q_tok, kmean_tok], [pgtok])
                            tt("dve", g_sb, pg[:, 0:16], cst["gmask"][:, own, :], ALU.add, [pgtok, c_tok], [g_tok])
                            P.op("dve", lambda h, top8=top8, g_sb=g_sb: h.max(top8, g_sb), [g_tok], [top_tok])
                            ts("dve", g_sb, g_sb, top8[:, 2:3], None, ALU.is_ge, None, [g_tok, top_tok], [g_tok])
                            tt("dve", g_sb, g_sb, cst["gvalid"][:, own, :], ALU.mult, [g_tok, c_tok], [g_tok])
                            tt("dve", sel[:, i, :], g_sb, cst["gown"][:, own, :], ALU.add, [g_tok, c_tok], [sel_tok[i]])
                            for nb in range(own + 1):
                                jl = [j for j in (2 * nb, 2 * nb + 1) if j <= qi]
                                for j in jl:
                                    jobs.append(dict(i=i, qi=qi, nb=nb, j=j, first=(j == jl[0]), last=(j == jl[-1]),
                                                     fin=(nb == own and j == jl[-1])))

                        def stage1(jb):
                            i, qi, j = jb["i"], jb["qi"], jb["j"]
                            qs = slice(i * 128, (i + 1) * 128)
                            pst, pstok = psA.get()
                            mm(pst[:, 0:128], kTh[:, j * 128:(j + 1) * 128], qT_sb[:, qs], True, True, [kTh_tok, q_tok], [pstok])
                            pT, pTtok = pT_ring.get()
                            act(pT, pst[:, 0:128], AF.Exp, [pstok, c_tok], [pTtok], scale=128.0 ** -0.5,
                                bias=cst["btab"][:, hd, qi - j:qi - j + 1])
                            if j == qi:
                                tt("pool", pT, pT, causal_b[:], ALU.mult, [pTtok, c_tok], [pTtok])
                            jb["pT"] = (pT, pTtok)

                        cur_po = {}

                        def stage2(jb):
                            i, nb, j = jb["i"], jb["nb"], jb["j"]
                            qs = slice(i * 128, (i + 1) * 128)
                            if jb["first"]:
                                cur_po["po"] = psB.get()
                            po, potok = cur_po["po"]
                            pT, pTtok = jb["pT"]
                            mm(po[:, 0:129], pT, vh[:, j, 0:129], jb["first"], jb["last"], [pTtok, vh_tok_sb], [potok])
                            if jb["last"]:
                                if nb == 0:
                                    ts("dve", acc[:, i, 0:129], po[:, 0:129], sel[:, i, 0:1], None, ALU.mult, None,
                                       [potok, sel_tok[i]], [acc_tok[i]])
                                else:
                                    stt(acc[:, i, 0:129], po[:, 0:129], sel[:, i, nb:nb + 1], acc[:, i, 0:129], ALU.mult, ALU.add,
                                        [potok, sel_tok[i], acc_tok[i]], [acc_tok[i]])
                            if jb["fin"]:
                                P.op("dve", lambda h, r=rec[:, i:i + 1], a=acc[:, i, 128:129]: h.reciprocal(r, a), [acc_tok[i]], [rec_tok])
                                ts("dve", o_sb, acc[:, i, 0:128], rec[:, i:i + 1], None, ALU.mult, None, [acc_tok[i], rec_tok], [o_tok])
                                ptr, ptrtok = psC.get()
                                transpose(ptr[:, 0:128], o_sb, cst["ident"][:], [o_tok, c_tok], [ptrtok])
                                cp("act", catT[:, hd, qs], ptr[:, 0:128], [ptrtok], [cat_tok[hd]])

                        LA = 3
                        for idx in range(len(jobs) + LA):
                            if idx < len(jobs):
                                stage1(jobs[idx])
                            if idx - LA >= 0:
                                stage2(jobs[idx - LA])
                    qm_base = 36

                chk(4)
                for a in range(4):
                    wb, wtok = load_w(("in", qm_base + a))
                    pt, ptok = proj_fm(wb, wtok)
                    cp("act", qm_sb, pt[:, :], [ptok], [qm_tok])
                    for mt in range(2):
                        pst, pstok = psC.get()
                        mm(pst[:, :], memk[:, a, mt * 128:(mt + 1) * 128], qm_sb, True, True, [memkv_tok, qm_tok], [pstok])
                        act(pm_sb[mt], pst[:, :], AF.Exp, [pstok], [pm_tok[mt]], scale=128.0 ** -0.5)
                    po, potok = psB.get()
                    pd, pdtok = psB.get()
                    for mt in range(2):
                        mm(po[:, :], memv[:, mt, a * 128:(a + 1) * 128], pm_sb[mt], mt == 0, mt == 1, [memkv_tok, pm_tok[mt]], [potok])
                    for mt in range(2):
                        mm(pd[:, :], ones_b[:], pm_sb[mt], mt == 0, mt == 1, [c_tok, pm_tok[mt]], [pdtok])
                    P.op("dve", lambda h, rden=rden, pd=pd: h.reciprocal(rden, pd[:, :]), [pdtok], [rden_tok])
                    tt("dve", catT[:, 12 + a, :], po[:, :], rden, ALU.mult, [potok, rden_tok], [cat_tok[12 + a]])

                chk(5)
                for c in range(NK):
                    wb, wtok = load_w(("o", c))
                    pt, ptok = proj_fm(wb, wtok, rhs=catT, rhs_toks=cat_tok)
                    tt("dve", x_sb[:, c, :], x_sb[:, c, :], pt[:, :], ALU.add, [ptok, x_tok[c]], [x_tok[c]])

                chk(6)
                rmsnorm_to_h(2 + 2 * li, TW)
                actT = fb["actT"]
                for ps_ in range(4):
                    for jj in range(11):
                        f = ps_ * 11 + jj
                        res = []
                        for gu in range(2):
                            ch = f + 44 * gu
                            wb, wtok = load_w(("up", ch))
                            pt, ptok = proj_fm(wb, wtok)
                            ext, etok = fb["ext"][gu], fb["ext_tok"][gu]
                            tbuf, ttok = (fb["tg"], fb["tg_tok"]) if gu == 0 else (fb["tu"], fb["tu_tok"])
                            cp("pool", ext[:, 0:2], halo_f[:, ch, :], [halo_tok[ch]], [etok])
                            cp("act", ext[:, 2:TW + 2], pt[:, :], [ptok], [etok])
                            cp("pool", halo_f[:, ch, :], ext[:, TW:TW + 2], [etok], [halo_tok[ch]])
                            act(tbuf, ext[:, 2:TW + 2], AF.Identity, [etok, small_tok], [ttok],
                                scale=cw_sb[:, 2, ch:ch + 1], bias=cb_sb[:, ch:ch + 1])
                            stt(tbuf, ext[:, 1:TW + 1], cw_sb[:, 1, ch:ch + 1], tbuf, ALU.mult, ALU.add, [etok, ttok], [ttok])
                            stt(tbuf, ext[:, 0:TW], cw_sb[:, 0, ch:ch + 1], tbuf, ALU.mult, ALU.add, [etok, ttok], [ttok])
                        act(fb["tg"], fb["tg"], AF.Silu, [fb["tg_tok"]], [fb["tg_tok"]])
                        tt("dve", actT[:, jj, :], fb["tg"], fb["tu"], ALU.mult, [fb["tg_tok"], fb["tu_tok"]], [fb["a_tok"][jj]])
                    for c in range(NK):
                        wb, wtok = load_w(("dn", c, ps_))
                        pt, ptok = psA.get()
                        for k in range(11):
                            mm(pt[:, :], wb[:, k, :], actT[:, k, :], k == 0, k == 10, [wtok, fb["a_tok"][k]], [ptok])
                        tt("dve", x_sb[:, c, :], x_sb[:, c, :], pt[:, :], ALU.add, [ptok, x_tok[c]], [x_tok[c]])

                chk(7)
                if last and do_final:
                    pst, pstok = psC.get()
                    for c in range(NK):
                        sq, sqt = sq_ring.get()
                        act(sq[:], x_sb[:, c, :], AF.Square, [x_tok[c]], [sqt])
                        mm(pst[:, :], ones_f[:], sq[:], c == 0, c == NK - 1, [sqt, c_tok], [pstok])
                    ts("dve", rstd[:], pst[:, :], 1.0 / D, EPS, ALU.mult, ALU.add, [pstok], [rstd_tok])
                    act(rstd[:], rstd[:], AF.Sqrt, [rstd_tok], [rstd_tok])
                    P.op("dve", lambda h: h.reciprocal(rstd[:], rstd[:]), [rstd_tok], [rstd_tok])
                    for c in range(NK):
                        stt(x_sb[:, c, :], x_sb[:, c, :], gains[:, 2 * L + 1, c:c + 1], rstd[:], ALU.mult, ALU.mult,
                            [x_tok[c], rstd_tok, c_tok], [x_tok[c]])
                dst_d = out_d if last else xs_d
                dma("act", dst_d[:, t0:t0 + TW].rearrange("(c p) t -> p c t", p=128), x_sb[:], x_tok, [xs_tok])

    except _Stop:
        pass
    P.emit()
    es.close()
    return nc


cst_cdec = make_consts()["cdec"]
INPUT_NAMES = (
    "x",
    "mem",
    "mem_norm",
    "l0_norm_mix",
    "l0_w_in",
    "l0_ret_gn",
    "l0_w_mem_kv",
    "l0_w_o",
    "l0_norm_ffn",
    "l0_ffn_w_up",
    "l0_ffn_conv_w",
    "l0_ffn_conv_b",
    "l0_ffn_w_down",
    "l1_norm_mix",
    "l1_w_in",
    "l1_conv_w",
    "l1_w_mem_kv",
    "l1_w_o",
    "l1_norm_ffn",
    "l1_ffn_w_up",
    "l1_ffn_conv_w",
    "l1_ffn_conv_b",
    "l1_ffn_w_down",
    "l2_norm_mix",
    "l2_w_in",
    "l2_w_mem_kv",
    "l2_w_o",
    "l2_norm_ffn",
    "l2_ffn_w_up",
    "l2_ffn_conv_w",
    "l2_ffn_conv_b",
    "l2_ffn_w_down",
    "l3_norm_mix",
    "l3_w_in",
    "l3_ret_gn",
    "l3_w_mem_kv",
    "l3_w_o",
    "l3_norm_ffn",
    "l3_ffn_w_up",
    "l3_ffn_conv_w",
    "l3_ffn_conv_b",
    "l3_ffn_w_down",
    "final_norm",
)
_CACHE = {}


def host_inputs(inputs, L=4):
    c = make_consts()
    shared = {}
    for k in CONST_SHAPES:
        shared["c_" + k] = np.ascontiguousarray(c[k], dtype=np.float32)
    g = [vec16(inputs["mem_norm"])]
    for li in range(L):
        g.append(vec16(inputs["l%d_norm_mix" % li]))
        g.append(vec16(inputs["l%d_norm_ffn" % li]))
    g.append(vec16(inputs["final_norm"]))
    shared["gains"] = np.ascontiguousarray(np.stack(g, axis=1))
    for li in range(L):
        kind = KINDS[li]
        p = "l%d_" % li
        shared[p + "w_in"] = wchunks(inputs[p + "w_in"])
        shared[p + "w_kv"] = wchunks(inputs[p + "w_mem_kv"])
        shared[p + "w_o"] = wchunks(inputs[p + "w_o"])
        shared[p + "w_up"] = wchunks(inputs[p + "ffn_w_up"])
        wd = wchunks(inputs[p + "ffn_w_down"])
        shared[p + "w_dn"] = np.ascontiguousarray(wd.reshape(16, 128, 4, 11, 128).transpose(0, 2, 1, 3, 4))
        cw = np.asarray(inputs[p + "ffn_conv_w"], np.float32)
        shared[p + "cw"] = np.ascontiguousarray(cw.reshape(3, 88, 128).transpose(2, 0, 1))
        cb = np.asarray(inputs[p + "ffn_conv_b"], np.float32)
        shared[p + "cb"] = np.ascontiguousarray(cb.reshape(88, 128).T)
        if kind == 0:
            shared[p + "gn"] = np.ascontiguousarray(np.asarray(inputs[p + "ret_gn"], np.float32).reshape(12, 128).T)
        if kind == 1:
            sc = np.asarray(inputs[p + "conv_w"], np.float32)
            shared[p + "scw"] = np.ascontiguousarray(sc.reshape(3, 12, 128).transpose(2, 0, 1))
    return shared


def kernel(**inputs):
    for nm in INPUT_NAMES:
        assert nm in inputs, nm
    x = np.asarray(inputs["x"], np.float32)
    mem = np.asarray(inputs["mem"], np.float32)
    B, S, _ = x.shape
    key = (S,)
    if key not in _CACHE:
        _CACHE[key] = build(S)
    nc = _CACHE[key]
    shared = host_inputs(inputs)
    in_maps = []
    for cidx in range(8):
        b = cidx % B
        m = dict(shared)
        m["xT"] = np.ascontiguousarray(x[b].T)
        m["memT"] = np.ascontiguousarray(mem[b].T)
        in_maps.append(m)
    res = run_bass_kernel_spmd(nc, in_maps, core_ids=list(range(8)))
    out = np.stack([np.ascontiguousarray(res.results[b]["outT"].T) for b in range(B)], axis=0)
    return out.astype(np.float32)
```

```python
from contextlib import ExitStack
import math
import numpy as np
import ml_dtypes
import concourse.bass as bass
import concourse.mybir as mybir
from concourse.bass_utils import run_bass_kernel_spmd

F32 = mybir.dt.float32
BF16 = mybir.dt.bfloat16
AF = mybir.ActivationFunctionType
ALU = mybir.AluOpType
AX = mybir.AxisListType

D = 2048
NK = 16
FF = 5632
NF = 44
EPS = 1e-6
MEM = 256
TW = 512
KINDS = (0, 1, 2, 0)
NCOLS = (6656, 5120, 5120, 6656)

ENGS = ["pe", "act", "dve", "pool", "sp"]
N_DMA_SEMS = 16


class Tok:
    __slots__ = ("last_w", "readers", "excl")

    def __init__(self, excl=False):
        self.last_w = None
        self.readers = []
        self.excl = excl


class Op:
    __slots__ = ("eng", "pos", "fn", "waits", "is_dma", "dma_sem", "dma_val", "waited", "sig")

    def __init__(self, eng, pos, fn, is_dma):
        self.eng = eng
        self.pos = pos
        self.fn = fn
        self.waits = []
        self.is_dma = is_dma
        self.dma_sem = None
        self.dma_val = None
        self.waited = False
        self.sig = None


class Prog:
    def __init__(self, nc):
        self.nc = nc
        self.streams = {e: [] for e in ENGS}
        self.seen = {e: {} for e in ENGS}
        self.dma_count = {e: 0 for e in ENGS}
        self.dma_ops = {e: [] for e in ENGS}
        self.pending = {e: [] for e in ENGS}

    def op(self, eng, fn, reads=(), writes=(), dma=False):
        st = self.streams[eng]
        o = Op(eng, len(st), fn, dma)
        if any(t.excl for t in reads):
            writes = list(writes) + [t for t in reads if t.excl]
            reads = [t for t in reads if not t.excl]
        deps = self.pending[eng]
        self.pending[eng] = []
        if dma:
            k = self.dma_count[eng]
            self.dma_count[eng] = k + 1
            o.dma_sem = k % N_DMA_SEMS
            o.dma_val = 16 * (k // N_DMA_SEMS + 1)
            self.dma_ops[eng].append(o)
            if k >= N_DMA_SEMS:
                deps.append(self.dma_ops[eng][k - N_DMA_SEMS])
        for t in reads:
            if t.last_w is not None:
                deps.append(t.last_w)
        for t in writes:
            if t.last_w is not None:
                deps.append(t.last_w)
            deps.extend(t.readers)
        seen = self.seen[eng]
        for d in deps:
            if d is o:
                continue
            if d.is_dma:
                key = ("dma", d.eng, d.dma_sem)
                val = d.dma_val
            else:
                if d.eng == eng and eng == "pe":
                    continue
                key = ("eng", d.eng)
                val = d.pos
            if seen.get(key, -1) >= val:
                continue
            seen[key] = val
            o.waits.append(d)
            d.waited = True
        for t in reads:
            t.readers.append(o)
        for t in writes:
            t.last_w = o
            t.readers = []
        st.append(o)
        return o

    def barrier(self):
        lasts = []
        for e in ("pe", "act", "dve", "pool"):
            for o in reversed(self.streams[e]):
                if not o.is_dma:
                    lasts.append(o)
                    break
            lasts.extend(self.dma_ops[e][-N_DMA_SEMS:])
        for e in ("pe", "act", "dve", "pool"):
            self.pending[e] = list(lasts)

    def emit(self):
        nc = self.nc
        with ExitStack() as es:
            esem = {e: es.enter_context(nc.semaphore("s_" + e)) for e in ENGS}
            dsem = {e: [es.enter_context(nc.semaphore("d_%s%d" % (e, i))) for i in range(N_DMA_SEMS)]
                    for e in ENGS if self.dma_count[e] > 0}
            for e in ENGS:
                c = 0
                for o in self.streams[e]:
                    if (not o.is_dma) and o.waited:
                        c += 1
                        o.sig = c
            block = es.enter_context(nc.Block())

            def run(engname, h):
                for o in self.streams[engname]:
                    for d in o.waits:
                        if d.is_dma:
                            h.wait_ge(dsem[d.eng][d.dma_sem], d.dma_val)
                        else:
                            h.wait_ge(esem[d.eng], d.sig)
                    ins = o.fn(h)
                    if o.is_dma:
                        ins.then_inc(dsem[engname][o.dma_sem], 16)
                    elif o.waited:
                        ins.then_inc(esem[engname], 1)
                if engname == "sp":
                    for e2 in dsem:
                        n = self.dma_count[e2]
                        for i in range(N_DMA_SEMS):
                            cnt = (n - i + N_DMA_SEMS - 1) // N_DMA_SEMS if n > i else 0
                            if cnt > 0:
                                h.wait_ge(dsem[e2][i], 16 * cnt)

            @block.tensor
            def _(h):
                run("pe", h)

            @block.scalar
            def _(h):
                run("act", h)

            @block.vector
            def _(h):
                run("dve", h)

            @block.gpsimd
            def _(h):
                run("pool", h)

            @block.sync
            def _(h):
                run("sp", h)


class Ring:
    def __init__(self, bufs, excl=False):
        self.bufs = bufs
        self.toks = [Tok(excl) for _ in bufs]
        self.i = 0

    def get(self):
        j = self.i % len(self.bufs)
        self.i += 1
        return self.bufs[j], self.toks[j]


def alibi_slopes(n):
    def pow2(m):
        start = 2.0 ** (-8.0 / m)
        return [start ** (i + 1) for i in range(m)]
    if math.log2(n).is_integer():
        return pow2(n)
    c = 2 ** int(math.floor(math.log2(n)))
    return pow2(c) + list(alibi_slopes(2 * c))[0::2][: n - c]


def make_consts():
    c = {}
    c["ident"] = np.eye(128, dtype=np.float32)
    lg = np.log1p(-np.exp2(-5.0 - np.arange(6, dtype=np.float64)))
    i = np.arange(128, dtype=np.float64)
    diff = i[:, None] - i[None, :]
    intra = np.where(diff >= 0, np.exp(np.maximum(diff, 0.0)[None] * lg[:, None, None]), 0.0)
    c["intraT"] = np.ascontiguousarray((intra / 16.0).transpose(2, 0, 1)).astype(np.float32)
    qd = np.exp((i + 1.0)[None] * lg[:, None])
    c["qdec"] = np.ascontiguousarray(np.broadcast_to(qd[None], (128, 6, 128))).astype(np.float32)
    kd = np.exp((127.0 - i)[None] * lg[:, None]) / 16.0
    c["kdec"] = np.ascontiguousarray(kd.T).astype(np.float32)
    c["cdec"] = [float(np.exp(128.0 * v)) for v in lg]
    sl = np.asarray(alibi_slopes(12), dtype=np.float64)
    p = np.arange(128, dtype=np.float64)
    dl = np.arange(32, dtype=np.float64)
    c["btab"] = (-sl[None, :, None] * (128.0 * dl[None, None, :] + 63.5 - p[:, None, None])).astype(np.float32)
    c["causal"] = (p[:, None] <= p[None, :]).astype(np.float32)
    own = np.arange(16)[:, None]
    n = np.arange(16)[None, :]
    gm = np.where(n < own, 0.0, -1e30).astype(np.float32)
    va = (n < own).astype(np.float32)
    oh = (n == own).astype(np.float32)
    c["gmask"] = np.ascontiguousarray(np.broadcast_to(gm[None], (128, 16, 16)))
    c["gvalid"] = np.ascontiguousarray(np.broadcast_to(va[None], (128, 16, 16)))
    c["gown"] = np.ascontiguousarray(np.broadcast_to(oh[None], (128, 16, 16)))
    return c


CONST_SHAPES = {"ident": [128, 128], "intraT": [128, 6, 128], "qdec": [128, 6, 128], "kdec": [128, 6],
                "btab": [128, 12, 32], "causal": [128, 128], "gmask": [128, 16, 16], "gvalid": [128, 16, 16],
                "gown": [128, 16, 16]}


def vec16(v):
    return np.ascontiguousarray(np.asarray(v, np.float32).reshape(16, 128).T)


def wchunks(w):
    w = np.asarray(w, np.float32)
    K, N = w.shape
    return np.ascontiguousarray(w.reshape(K // 128, 128, N // 128, 128).transpose(2, 1, 0, 3))


class _Stop(Exception):
    pass


def build(T, kinds=KINDS, do_final=True, stop=0):
    nc = bass.Bass("TRN2", target_bir_lowering=False)
    L = len(kinds)
    NW = T // TW
    NT = T // 128
    P = Prog(nc)
    es = ExitStack()

    def din(name, shape, dt=F32):
        return nc.dram_tensor(name, list(shape), dt, kind="ExternalInput").ap()

    xT_in = din("xT", [D, T])
    memT_in = din("memT", [D, MEM])
    gains_in = din("gains", [128, 2 * L + 2, 16])
    cst_in = {k: din("c_" + k, s) for k, s in CONST_SHAPES.items()}
    lw = []
    for li, kind in enumerate(kinds):
        nch = NCOLS[kind if kind < 3 else 0] // 128
        d = {"w_in": din("l%d_w_in" % li, [nch, 128, 16, 128]),
             "w_kv": din("l%d_w_kv" % li, [8, 128, 16, 128]),
             "w_o": din("l%d_w_o" % li, [16, 128, 16, 128]),
             "w_up": din("l%d_w_up" % li, [88, 128, 16, 128]),
             "w_dn": din("l%d_w_dn" % li, [16, 4, 128, 11, 128]),
             "cw": din("l%d_cw" % li, [128, 3, 88]),
             "cb": din("l%d_cb" % li, [128, 88])}
        if kind == 0:
            d["gn"] = din("l%d_gn" % li, [128, 12])
        if kind == 1:
            d["scw"] = din("l%d_scw" % li, [128, 3, 12])
        lw.append(d)
    out_d = nc.dram_tensor("outT", [D, T], F32, kind="ExternalOutput").ap()
    xs_d = nc.dram_tensor("xscr", [D, T], F32, kind="Internal").ap()
    kh_d = nc.dram_tensor("khist", [12, 128, T], BF16, kind="Internal").ap()
    vh_d = nc.dram_tensor("vhist", [12, 128, NT, 130], BF16, kind="Internal").ap()
    NCHMAX = 228
    wsc_d = nc.dram_tensor("wsc", [2, NCHMAX, 128, 16, 128], BF16, kind="Internal").ap()
    wsc_tok = [[Tok() for _ in range(NCHMAX)] for _ in range(2)]
    chunks = []
    widx = []
    for li_, kind_ in enumerate(kinds):
        W_ = lw[li_]
        cl = []
        for j in range(8):
            cl.append((("kv", j), W_["w_kv"][j], 16))
        for j in range(NCOLS[kind_] // 128):
            cl.append((("in", j), W_["w_in"][j], 16))
        for j in range(16):
            cl.append((("o", j), W_["w_o"][j], 16))
        for j in range(88):
            cl.append((("up", j), W_["w_up"][j], 16))
        for j in range(16):
            for p_ in range(4):
                cl.append((("dn", j, p_), W_["w_dn"][j][p_], 11))
        chunks.append(cl)
        widx.append({k: (i, kc) for i, (k, _, kc) in enumerate(cl)})
    kh_tok = [Tok() for _ in range(12)]
    vh_tok = [Tok() for _ in range(12)]
    xs_tok = Tok()

    def sb(name, shape, dt):
        return es.enter_context(nc.sbuf_tensor("sb_" + name, list(shape), dt))

    def ps(name, dt=F32, n=512):
        return es.enter_context(nc.psum_tensor(name, [128, n], dt))

    ones_f = sb("ones_f", [128, 128], F32)
    ones_b = sb("ones_b", [128, 128], BF16)
    gains = sb("gains", [128, 2 * L + 2, 16], F32)
    cst = {k: sb("c_" + k, s, F32) for k, s in CONST_SHAPES.items()}
    causal_b = sb("causal_b", [128, 128], BF16)
    cw_sb = sb("cw", [128, 3, 88], F32)
    cb_sb = sb("cb", [128, 88], F32)
    gn_sb = sb("gn", [128, 12], F32)
    scw_sb = sb("scw", [128, 3, 12], F32)
    small_tok = Tok()
    x_sb = sb("x_sb", [128, NK, TW], F32)
    x_tok = [Tok() for _ in range(NK)]
    hT = sb("hT", [128, NK, TW], BF16)
    h_tok = [Tok() for _ in range(NK)]
    rstd = sb("rstd", [128, TW], F32)
    rstd_tok = Tok()
    sq_ring = Ring([sb("sq%d" % i, [128, TW], F32) for i in range(2)])
    stg_ring = Ring([sb("stg%d" % i, [128, 16, 128], F32) for i in range(2)])
    wbf_ring = Ring([sb("wbf%d" % i, [128, 16, 128], BF16) for i in range(4)])
    halo_f = sb("halo_f", [128, 88, 2], F32)
    halo_tok = [Tok() for _ in range(88)]
    halo_s = sb("halo_s", [128, 12, 2], F32)
    halos_tok = [Tok() for _ in range(12)]
    mem_nT = sb("mem_nT", [128, NK, MEM], BF16)
    memn_tok = Tok()
    memk = sb("memk", [128, 4, MEM], BF16)
    memv = sb("memv", [128, 2, 512], BF16)
    memkv_tok = Tok()
    kmean = sb("kmean", [128, 12, 16], BF16)
    kmean_tok = Tok()
    ABF = 63 * TW
    AF32 = 14 * TW
    arena_b = sb("arena_b", [128, ABF], BF16)
    arena_f = sb("arena_f", [128, AF32], F32)

    class Carver:
        def __init__(self, t, n):
            self.t, self.n, self.o = t, n, 0

        def take(self, shape):
            sz = int(np.prod(shape))
            assert self.o + sz <= self.n, (self.o, sz, self.n)
            v = self.t[:, self.o:self.o + sz]
            self.o += sz
            if len(shape) == 2:
                v = v.rearrange("p (a b) -> p a b", a=shape[0])
            elif len(shape) == 3:
                v = v.rearrange("p (a b c) -> p a b c", a=shape[0], b=shape[1])
            return v

    psA = Ring([ps("psA%d" % i) for i in range(4)], excl=True)
    psB = Ring([ps("psB%d" % i) for i in range(2)], excl=True)
    psC = Ring([ps("psC%d" % i) for i in range(2)], excl=True)

    def mm(out, lhsT, rhs, start, stop, reads, writes):
        P.op("pe", lambda h: h.matmul(out, lhsT, rhs, start=start, stop=stop), reads, writes)

    def transpose(out, in_, ident, reads, writes):
        P.op("pe", lambda h: h.transpose(out, in_, ident), reads, writes)

    def act(out, in_, func, reads, writes, scale=None, bias=None):
        kw = {}
        if scale is not None:
            kw["scale"] = scale
        if bias is not None:
            kw["bias"] = bias
        P.op("act", lambda h: h.activation(out, in_, func, **kw), reads, writes)

    def tt(eng, out, a, b, op, reads, writes):
        P.op(eng, lambda h: h.tensor_tensor(out, a, b, op), reads, writes)

    def ts(eng, out, a, s1, s2, op0, op1, reads, writes):
        if op1 is None:
            P.op(eng, lambda h: h.tensor_scalar(out, a, s1, None, op0), reads, writes)
        else:
            P.op(eng, lambda h: h.tensor_scalar(out, a, s1, s2, op0, op1), reads, writes)

    def stt(out, a, s, b, op0, op1, reads, writes):
        P.op("dve", lambda h: h.scalar_tensor_tensor(out, a, s, b, op0, op1), reads, writes)

    def cp(eng, out, in_, reads, writes):
        if eng == "act":
            P.op("act", lambda h: h.activation(out, in_, AF.Copy), reads, writes)
        else:
            P.op(eng, lambda h: h.tensor_copy(out, in_), reads, writes)

    def dma(q, out, in_, reads, writes):
        P.op(q, lambda h: h.dma_start(out=out, in_=in_), reads, writes, dma=True)

    cur = {"li": 0, "w": 0, "pc0": 0, "nc": 0}

    def precast(li_, i0, i1):
        buf = li_ % 2
        for i in range(i0, i1):
            _, src, kc = chunks[li_][i]
            dma("pool", wsc_d[buf][i][:, 0:kc, :], src, [], [wsc_tok[buf][i]])

    def load_w(key):
        li_ = cur["li"]
        i, kc = widx[li_][key]
        wb, wtok = wbf_ring.get()
        if li_ == 0 and cur["w"] == 0:
            _, src, _ = chunks[0][i]
            stg, stok = stg_ring.get()
            dma("sp", stg[:, 0:kc, :], src, [], [stok])
            ceng = ("pool", "dve", "act")[cur["nc"] % 3]
            cur["nc"] += 1
            cp(ceng, wb[:, 0:kc, :], stg[:, 0:kc, :], [stok], [wtok])
            dma("pool", wsc_d[0][i][:, 0:kc, :], wb[:, 0:kc, :], [wtok], [wsc_tok[0][i]])
        else:
            assert wsc_tok[li_ % 2][i].last_w is not None, (li_, key, i)
            dma("sp", wb[:, 0:kc, :], wsc_d[li_ % 2][i][:, 0:kc, :], [wsc_tok[li_ % 2][i]], [wtok])
        return wb, wtok

    c_tok = Tok()
    P.op("pool", lambda h: h.memset(ones_f[:], 1.0), [], [c_tok])
    P.op("pool", lambda h: h.memset(ones_b[:], 1.0), [], [c_tok])
    P.op("pool", lambda h: h.memset(kmean[:], 0.0), [], [kmean_tok])
    P.op("pool", lambda h: h.memset(halo_f[:], 0.0), [], halo_tok)
    P.op("pool", lambda h: h.memset(halo_s[:], 0.0), [], halos_tok)
    dma("sp", gains[:], gains_in, [], [c_tok])
    for k in CONST_SHAPES:
        dma("sp", cst[k][:], cst_in[k], [], [c_tok])
    cp("pool", causal_b[:], cst["causal"][:], [c_tok], [c_tok])

    def rmsnorm_to_h(gidx, n, src=None, src_toks=None, dst=None, dst_toks=None):
        src = x_sb if src is None else src
        src_toks = x_tok if src_toks is None else src_toks
        dst = hT if dst is None else dst
        dst_toks = h_tok if dst_toks is None else dst_toks
        pst, ptok = psC.get()
        for c in range(NK):
            sq, sqt = sq_ring.get()
            act(sq[:, 0:n], src[:, c, 0:n], AF.Square, [src_toks[c]], [sqt])
            mm(pst[:, 0:n], ones_f[:], sq[:, 0:n], c == 0, c == NK - 1, [sqt, c_tok], [ptok])
        ts("dve", rstd[:, 0:n], pst[:, 0:n], 1.0 / D, EPS, ALU.mult, ALU.add, [ptok], [rstd_tok])
        act(rstd[:, 0:n], rstd[:, 0:n], AF.Sqrt, [rstd_tok], [rstd_tok])
        P.op("dve", lambda h: h.reciprocal(rstd[:, 0:n], rstd[:, 0:n]), [rstd_tok], [rstd_tok])
        for c in range(NK):
            stt(dst[:, c, 0:n], src[:, c, 0:n], gains[:, gidx, c:c + 1], rstd[:, 0:n], ALU.mult, ALU.mult,
                [src_toks[c], rstd_tok, c_tok], [dst_toks[c]])

    dma("sp", x_sb[:, :, 0:MEM], memT_in.rearrange("(c p) t -> p c t", p=128), [], x_tok)
    rmsnorm_to_h(0, MEM, dst=mem_nT, dst_toks=[memn_tok] * NK)

    def proj_fm(wb, wtok, n=TW, rhs=None, rhs_toks=None):
        rhs = hT if rhs is None else rhs
        rhs_toks = h_tok if rhs_toks is None else rhs_toks
        pt, ptok = psA.get()
        for k in range(NK):
            mm(pt[:, 0:n], wb[:, k, :], rhs[:, k, 0:n], k == 0, k == NK - 1, [wtok, rhs_toks[k]], [ptok])
        return pt, ptok

    def proj_tm(pt, ptok, col0, wb, wtok, tile, lhs=None, lhs_toks=None):
        lhs = hT if lhs is None else lhs
        lhs_toks = h_tok if lhs_toks is None else lhs_toks
        for k in range(NK):
            mm(pt[:, col0:col0 + 128], lhs[:, k, tile * 128:(tile + 1) * 128], wb[:, k, :], k == 0, k == NK - 1,
               [wtok, lhs_toks[k]], [ptok])

    def chk(n):
        if stop == n:
            dma("act", out_d[:, 0:TW].rearrange("(c p) t -> p c t", p=128), x_sb[:], x_tok, [xs_tok])
            raise _Stop()

    try:
        for li, kind in enumerate(kinds):
            W = lw[li]
            src_d = xT_in if li == 0 else xs_d
            last = (li == L - 1)
            chk(1)
            cur["li"] = li
            cur["w"] = 0
            P.barrier()
            dma("sp", cw_sb[:], W["cw"], [], [small_tok])
            dma("sp", cb_sb[:], W["cb"], [], [small_tok])
            if kind == 0:
                dma("sp", gn_sb[:], W["gn"], [], [small_tok])
            if kind == 1:
                dma("sp", scw_sb[:], W["scw"], [], [small_tok])
            P.op("pool", lambda h: h.memset(halo_f[:], 0.0), [], halo_tok)
            for j in range(4):
                wb, wtok = load_w(("kv", j))
                pt, ptok = proj_fm(wb, wtok, n=MEM, rhs=mem_nT, rhs_toks=[memn_tok] * NK)
                cp("act", memk[:, j, :], pt[:, 0:MEM], [ptok], [memkv_tok])
            for mt in range(2):
                pt, ptok = psA.get()
                for j in range(4):
                    wb, wtok = load_w(("kv", 4 + j))
                    proj_tm(pt, ptok, j * 128, wb, wtok, mt, lhs=mem_nT, lhs_toks=[memn_tok] * NK)
                cp("act", memv[:, mt, :], pt[:, :], [ptok], [memkv_tok])

            P.barrier()
            P.op("pool", lambda h: h.memset(arena_b[:], 0.0), [], [])
            P.op("pool", lambda h: h.memset(arena_f[:], 0.0), [], [])
            P.barrier()
            cb_ = Carver(arena_b, ABF)
            cf_ = Carver(arena_f, AF32)
            catT = cb_.take([NK, TW])
            cat_tok = [Tok() for _ in range(NK)]
            qm_sb = cb_.take([TW])
            qm_tok = Tok()
            pm_sb = [cb_.take([TW]) for _ in range(2)]
            pm_tok = [Tok(), Tok()]
            rden = cf_.take([TW])
            rden_tok = Tok()
            if kind == 0:
                qT_sb = cb_.take([2, TW]); qTd_sb = cb_.take([2, TW]); kT_sb = cb_.take([2, TW]); sg_sb = cb_.take([2, TW])
                q_tok, qd_tok, k_tok, sg_tok = Tok(), Tok(), Tok(), Tok()
                ktok_sb = cb_.take([4, 256]); vtok_sb = cb_.take([4, 256])
                kt_tok = [Tok() for _ in range(4)]; vt_tok = [Tok() for _ in range(4)]
                S_b = cb_.take([6, 2, 256]); Sb_tok = [Tok() for _ in range(6)]
                sT_sb = [cb_.take([128]) for _ in range(2)]; sT_ring = Ring(sT_sb)
                S_f = cf_.take([6, 2, 256]); Sf_tok = [Tok() for _ in range(6)]
                y_sb = cf_.take([2, TW]); y_tok = Tok()
            if kind == 1:
                c_sb = cf_.take([TW]); cs_tok = Tok()
                ext_s = cf_.take([TW + 2]); exts_tok = Tok()
                t_s = cf_.take([TW]); ts_tok = Tok()
                P.op("pool", lambda h: h.memset(halo_s[:], 0.0), [], halos_tok)
            if kind == 2:
                qT_l = [cb_.take([TW]) for _ in range(2)]; q_tl = [Tok(), Tok()]
                kT_l = [cb_.take([TW]) for _ in range(2)]; k_tl = [Tok(), Tok()]
                va_l = [cb_.take([4, 130]) for _ in range(2)]; va_tl = [Tok(), Tok()]
                kTh_l = [cb_.take([T]) for _ in range(2)]; kTh_tl = [Tok(), Tok()]
                vh = cb_.take([NT, 130]); vh_tok_sb = Tok()
                pT_ring = Ring([cb_.take([128]) for _ in range(5)])
                acc = cf_.take([4, 132]); acc_tok = [Tok() for _ in range(4)]
                g_sb = cf_.take([16]); g_tok = Tok()
                top8 = cf_.take([8]); top_tok = Tok()
                sel = cf_.take([4, 16]); sel_tok = [Tok() for _ in range(4)]
                rec = cf_.take([4]); rec_tok = Tok()
                o_sb = cf_.take([128]); o_tok = Tok()
                kred = cf_.take([2]); kred_tok = Tok()
                P.op("pool", lambda h: h.memset(kmean[:], 0.0), [], [kmean_tok])
                for va_sb, va_tok in zip(va_l, va_tl):
                    P.op("pool", lambda h, va_sb=va_sb: h.memset(va_sb, 1.0), [], [va_tok])
            fb = dict(actT=cb_.take([11, TW]), a_tok=[Tok() for _ in range(11)],
                      ext=[cf_.take([TW + 2]) for _ in range(2)], ext_tok=[Tok(), Tok()],
                      tg=cf_.take([TW]), tg_tok=Tok(), tu=cf_.take([TW]), tu_tok=Tok())

            for w in range(NW):
                t0 = w * TW
                chk(2)
                cur["w"] = w
                if li + 1 < L:
                    nchn = len(chunks[li + 1])
                    precast(li + 1, (w * nchn) // NW, ((w + 1) * nchn) // NW)
                dma("sp", x_sb[:], src_d[:, t0:t0 + TW].rearrange("(c p) t -> p c t", p=128), [xs_tok], x_tok)
                rmsnorm_to_h(1 + 2 * li, TW)

                chk(3)
                if kind == 0:
                    for hd in range(6):
                        for dk in range(2):
                            wb, wtok = load_w(("in", 2 * hd + dk))
                            pt, ptok = proj_fm(wb, wtok)
                            cp("act", qT_sb[:, dk, :], pt[:, :], [ptok], [q_tok])
                            for n4 in range(4):
                                tt("dve", qTd_sb[:, dk, n4 * 128:(n4 + 1) * 128], pt[:, n4 * 128:(n4 + 1) * 128],
                                   cst["qdec"][:, hd, :], ALU.mult, [ptok, c_tok], [qd_tok])
                        ptk = [psB.get() for _ in range(2)]
                        for dk in range(2):
                            wb, wtok = load_w(("in", 12 + 2 * hd + dk))
                            pt, ptok = proj_fm(wb, wtok)
                            cp("act", kT_sb[:, dk, :], pt[:, :], [ptok], [k_tok])
                            for tl in range(4):
                                proj_tm(ptk[tl // 2][0], ptk[tl // 2][1], (tl % 2) * 256 + dk * 128, wb, wtok, tl)
                        for tl in range(4):
                            ts("dve", ktok_sb[:, tl, :], ptk[tl // 2][0][:, (tl % 2) * 256:(tl % 2) * 256 + 256],
                               cst["kdec"][:, hd:hd + 1], None, ALU.mult, None, [ptk[tl // 2][1], c_tok], [kt_tok[tl]])
                        ptv = [psB.get() for _ in range(2)]
                        for dk in range(2):
                            wb, wtok = load_w(("in", 24 + 2 * hd + dk))
                            for tl in range(4):
                                proj_tm(ptv[tl // 2][0], ptv[tl // 2][1], (tl % 2) * 256 + dk * 128, wb, wtok, tl)
                        for tl in range(4):
                            cp("act", vtok_sb[:, tl, :], ptv[tl // 2][0][:, (tl % 2) * 256:(tl % 2) * 256 + 256],
                               [ptv[tl // 2][1]], [vt_tok[tl]])
                        for dk in range(2):
                            wb, wtok = load_w(("in", 36 + 2 * hd + dk))
                            pt, ptok = proj_fm(wb, wtok)
                            act(sg_sb[:, dk, :], pt[:, :], AF.Silu, [ptok], [sg_tok])
                        py = [psB.get() for _ in range(2)]
                        for n in range(4):
                            cs = slice(n * 128, (n + 1) * 128)
                            pst, pstok = psC.get()
                            for dk in range(2):
                                mm(pst[:, 0:128], kT_sb[:, dk, cs], qT_sb[:, dk, cs], dk == 0, dk == 1, [k_tok, q_tok], [pstok])
                            sT, sTtok = sT_ring.get()
                            tt("dve", sT, pst[:, 0:128], cst["intraT"][:, hd, :], ALU.mult, [pstok, c_tok], [sTtok])
                            for ec in range(2):
                                mm(py[ec][0][:, cs], vtok_sb[:, n, ec * 128:(ec + 1) * 128], sT, True, False,
                                   [vt_tok[n], sTtok], [py[ec][1]])
                                for dk in range(2):
                                    mm(py[ec][0][:, cs], S_b[:, hd, dk, ec * 128:(ec + 1) * 128], qTd_sb[:, dk, cs], False, dk == 1,
                                       [Sb_tok[hd], qd_tok], [py[ec][1]])
                            pkv, pkvtok = psC.get()
                            for dk in range(2):
                                mm(pkv[:, dk * 256:(dk + 1) * 256], ktok_sb[:, n, dk * 128:(dk + 1) * 128], vtok_sb[:, n, :], True, True,
                                   [kt_tok[n], vt_tok[n]], [pkvtok])
                            Sf = S_f[:, hd, :, :].rearrange("p a b -> p (a b)")
                            stt(Sf, Sf, cst_cdec[hd], pkv[:, :], ALU.mult, ALU.add, [pkvtok, Sf_tok[hd]], [Sf_tok[hd]])
                            cp("pool", S_b[:, hd, :, :].rearrange("p a b -> p (a b)"), Sf, [Sf_tok[hd]], [Sb_tok[hd]])
                        pst, pstok = psC.get()
                        for ec in range(2):
                            cp("act", y_sb[:, ec, :], py[ec][0][:, :], [py[ec][1]], [y_tok])
                            sq, sqt = sq_ring.get()
                            act(sq[:], py[ec][0][:, :], AF.Square, [py[ec][1]], [sqt])
                            mm(pst[:, :], ones_f[:], sq[:], ec == 0, ec == 1, [sqt, c_tok], [pstok])
                        ts("dve", rden, pst[:, :], 1.0 / 256, EPS, ALU.mult, ALU.add, [pstok], [rden_tok])
                        act(rden, rden, AF.Sqrt, [rden_tok], [rden_tok])
                        P.op("dve", lambda h, rden=rden: h.reciprocal(rden, rden), [rden_tok], [rden_tok])
                        for ec in range(2):
                            stt(y_sb[:, ec, :], y_sb[:, ec, :], gn_sb[:, 2 * hd + ec:2 * hd + ec + 1], rden, ALU.mult, ALU.mult,
                                [y_tok, rden_tok, small_tok], [y_tok])
                            tt("dve", catT[:, 2 * hd + ec, :], y_sb[:, ec, :], sg_sb[:, ec, :], ALU.mult, [y_tok, sg_tok],
                               [cat_tok[2 * hd + ec]])
                    qm_base = 48
                elif kind == 1:
                    for j in range(12):
                        wb, wtok = load_w(("in", 12 + j))
                        pc, pctok = proj_fm(wb, wtok)
                        cp("act", c_sb, pc[:, :], [pctok], [cs_tok])
                        wb, wtok = load_w(("in", 24 + j))
                        ph, phtok = proj_fm(wb, wtok)
                        cp("pool", ext_s[:, 0:2], halo_s[:, j, :], [halos_tok[j]], [exts_tok])
                        tt("dve", ext_s[:, 2:TW + 2], ph[:, :], c_sb, ALU.mult, [phtok, cs_tok], [exts_tok])
                        cp("pool", halo_s[:, j, :], ext_s[:, TW:TW + 2], [exts_tok], [halos_tok[j]])
                        act(t_s, ext_s[:, 2:TW + 2], AF.Identity, [exts_tok, small_tok], [ts_tok], scale=scw_sb[:, 2, j:j + 1])
                        stt(t_s, ext_s[:, 1:TW + 1], scw_sb[:, 1, j:j + 1], t_s, ALU.mult, ALU.add, [exts_tok, ts_tok], [ts_tok])
                        stt(t_s, ext_s[:, 0:TW], scw_sb[:, 0, j:j + 1], t_s, ALU.mult, ALU.add, [exts_tok, ts_tok], [ts_tok])
                        wb, wtok = load_w(("in", j))
                        pb, pbtok = proj_fm(wb, wtok)
                        tt("dve", catT[:, j, :], pb[:, :], t_s, ALU.mult, [pbtok, ts_tok], [cat_tok[j]])
                    qm_base = 36
                else:
                    for hd in range(12):
                        qT_sb, q_tok = qT_l[hd % 2], q_tl[hd % 2]
                        kT_sb, k_tok = kT_l[hd % 2], k_tl[hd % 2]
                        va_sb, va_tok = va_l[hd % 2], va_tl[hd % 2]
                        kTh, kTh_tok = kTh_l[hd % 2], kTh_tl[hd % 2]
                        wb, wtok = load_w(("in", hd))
                        pt, ptok = proj_fm(wb, wtok)
                        cp("act", qT_sb, pt[:, :], [ptok], [q_tok])
                        wb, wtok = load_w(("in", 12 + hd))
                        pt, ptok = proj_fm(wb, wtok)
                        cp("act", kT_sb, pt[:, :], [ptok], [k_tok])
                        P.op("dve", lambda h, kT_sb=kT_sb, kred=kred: h.tensor_reduce(
                            kred, kT_sb.rearrange("p (a b) -> p a b", a=2), AX.X, ALU.add), [k_tok], [kred_tok])
                        cp("pool", kmean[:, hd, 2 * w:2 * w + 2], kred, [kred_tok], [kmean_tok])
                        dma("act", kh_d[hd][:, t0:t0 + TW], kT_sb, [k_tok], [kh_tok[hd]])
                        wb, wtok = load_w(("in", 24 + hd))
                        pv, pvtok = psA.get()
                        for tl in range(4):
                            proj_tm(pv, pvtok, tl * 128, wb, wtok, tl)
                        cp("act", va_sb[:, :, 0:128], pv[:, :].rearrange("p (a b) -> p a b", a=4), [pvtok], [va_tok])
                        dma("act", vh_d[hd][:, 4 * w:4 * w + 4, :], va_sb, [va_tok], [vh_tok[hd]])
                        nkt = 4 * (w + 1)
                        dma("act", kTh[:, 0:t0 + TW], kh_d[hd][:, 0:t0 + TW], [kh_tok[hd]], [kTh_tok])
                        dma("act", vh[:, 0:nkt, :], vh_d[hd][:, 0:nkt, :], [vh_tok[hd]], [vh_tok_sb])
                        jobs = []
                        for i in range(4):
                            qi = 4 * w + i
                            own = qi // 2
                            qs = slice(i * 128, (i + 1) * 128)
                            pg, pgtok = psC.get()
                            mm(pg[:, 0:16], qT_sb[:, qs], kmean[:, hd, :], True, True, [q_tok, kmean_tok], [pgtok])
                            tt("dve", g_sb, pg[:, 0:16], cst["gmask"][:, own, :], ALU.add, [pgtok, c_tok], [g_tok])
                            P.op("dve", lambda h, top8=top8, g_sb=g_sb: h.max(top8, g_sb), [g_tok], [top_tok])
                            ts("dve", g_sb, g_sb, top8[:, 2:3], None, ALU.is_ge, None, [g_tok, top_tok], [g_tok])
                            tt("dve", g_sb, g_sb, cst["gvalid"][:, own, :], ALU.mult, [g_tok, c_tok], [g_tok])
                            tt("dve", sel[:, i, :], g_sb, cst["gown"][:, own, :], ALU.add, [g_tok, c_tok], [sel_tok[i]])
                            for nb in range(own + 1):
                                jl = [j for j in (2 * nb, 2 * nb + 1) if j <= qi]
                                for j in jl:
                                    jobs.append(dict(i=i, qi=qi, nb=nb, j=j, first=(j == jl[0]), last=(j == jl[-1]),
                                                     fin=(nb == own and j == jl[-1])))

                        def stage1(jb):
                            i, qi, j = jb["i"], jb["qi"], jb["j"]
                            qs = slice(i * 128, (i + 1) * 128)
                            pst, pstok = psA.get()
                            mm(pst[:, 0:128], kTh[:, j * 128:(j + 1) * 128], qT_sb[:, qs], True, True, [kTh_tok, q_tok], [pstok])
                            pT, pTtok = pT_ring.get()
                            act(pT, pst[:, 0:128], AF.Exp, [pstok, c_tok], [pTtok], scale=128.0 ** -0.5,
                                bias=cst["btab"][:, hd, qi - j:qi - j + 1])
                            if j == qi:
                                tt("pool", pT, pT, causal_b[:], ALU.mult, [pTtok, c_tok], [pTtok])
                            jb["pT"] = (pT, pTtok)

                        cur_po = {}

                        def stage2(jb):
                            i, nb, j = jb["i"], jb["nb"], jb["j"]
                            qs = slice(i * 128, (i + 1) * 128)
                            if jb["first"]:
                                cur_po["po"] = psB.get()
                            po, potok = cur_po["po"]
                            pT, pTtok = jb["pT"]
                            mm(po[:, 0:129], pT, vh[:, j, 0:129], jb["first"], jb["last"], [pTtok, vh_tok_sb], [potok])
                            if jb["last"]:
                                if nb == 0:
                                    ts("dve", acc[:, i, 0:129], po[:, 0:129], sel[:, i, 0:1], None, ALU.mult, None,
                                       [potok, sel_tok[i]], [acc_tok[i]])
                                else:
                                    stt(acc[:, i, 0:129], po[:, 0:129], sel[:, i, nb:nb + 1], acc[:, i, 0:129], ALU.mult, ALU.add,
                                        [potok, sel_tok[i], acc_tok[i]], [acc_tok[i]])
                            if jb["fin"]:
                                P.op("dve", lambda h, r=rec[:, i:i + 1], a=acc[:, i, 128:129]: h.reciprocal(r, a), [acc_tok[i]], [rec_tok])
                                ts("dve", o_sb, acc[:, i, 0:128], rec[:, i:i + 1], None, ALU.mult, None, [acc_tok[i], rec_tok], [o_tok])
                                ptr, ptrtok = psC.get()
                                transpose(ptr[:, 0:128], o_sb, cst["ident"][:], [o_tok, c_tok], [ptrtok])
                                cp("act", catT[:, hd, qs], ptr[:, 0:128], [ptrtok], [cat_tok[hd]])

                        LA = 3
                        for idx in range(len(jobs) + LA):
                            if idx < len(jobs):
                                stage1(jobs[idx])
                            if idx - LA >= 0:
                                stage2(jobs[idx - LA])
                    qm_base = 36

                chk(4)
                for a in range(4):
                    wb, wtok = load_w(("in", qm_base + a))
                    pt, ptok = proj_fm(wb, wtok)
                    cp("act", qm_sb, pt[:, :], [ptok], [qm_tok])
                    for mt in range(2):
                        pst, pstok = psC.get()
                        mm(pst[:, :], memk[:, a, mt * 128:(mt + 1) * 128], qm_sb, True, True, [memkv_tok, qm_tok], [pstok])
                        act(pm_sb[mt], pst[:, :], AF.Exp, [pstok], [pm_tok[mt]], scale=128.0 ** -0.5)
                    po, potok = psB.get()
                    pd, pdtok = psB.get()
                    for mt in range(2):
                        mm(po[:, :], memv[:, mt, a * 128:(a + 1) * 128], pm_sb[mt], mt == 0, mt == 1, [memkv_tok, pm_tok[mt]], [potok])
                    for mt in range(2):
                        mm(pd[:, :], ones_b[:], pm_sb[mt], mt == 0, mt == 1, [c_tok, pm_tok[mt]], [pdtok])
                    P.op("dve", lambda h, rden=rden, pd=pd: h.reciprocal(rden, pd[:, :]), [pdtok], [rden_tok])
                    tt("dve", catT[:, 12 + a, :], po[:, :], rden, ALU.mult, [potok, rden_tok], [cat_tok[12 + a]])

                chk(5)
                for c in range(NK):
                    wb, wtok = load_w(("o", c))
                    pt, ptok = proj_fm(wb, wtok, rhs=catT, rhs_toks=cat_tok)
                    tt("dve", x_sb[:, c, :], x_sb[:, c, :], pt[:, :], ALU.add, [ptok, x_tok[c]], [x_tok[c]])

                chk(6)
                rmsnorm_to_h(2 + 2 * li, TW)
                actT = fb["actT"]
                for ps_ in range(4):
                    for jj in range(11):
                        f = ps_ * 11 + jj
                        res = []
                        for gu in range(2):
                            ch = f + 44 * gu
                            wb, wtok = load_w(("up", ch))
                            pt, ptok = proj_fm(wb, wtok)
                            ext, etok = fb["ext"][gu], fb["ext_tok"][gu]
                            tbuf, ttok = (fb["tg"], fb["tg_tok"]) if gu == 0 else (fb["tu"], fb["tu_tok"])
                            cp("pool", ext[:, 0:2], halo_f[:, ch, :], [halo_tok[ch]], [etok])
                            cp("act", ext[:, 2:TW + 2], pt[:, :], [ptok], [etok])
                            cp("pool", halo_f[:, ch, :], ext[:, TW:TW + 2], [etok], [halo_tok[ch]])
                            act(tbuf, ext[:, 2:TW + 2], AF.Identity, [etok, small_tok], [ttok],
                                scale=cw_sb[:, 2, ch:ch + 1], bias=cb_sb[:, ch:ch + 1])
                            stt(tbuf, ext[:, 1:TW + 1], cw_sb[:, 1, ch:ch + 1], tbuf, ALU.mult, ALU.add, [etok, ttok], [ttok])
                            stt(tbuf, ext[:, 0:TW], cw_sb[:, 0, ch:ch + 1], tbuf, ALU.mult, ALU.add, [etok, ttok], [ttok])
                        act(fb["tg"], fb["tg"], AF.Silu, [fb["tg_tok"]], [fb["tg_tok"]])
                        tt("dve", actT[:, jj, :], fb["tg"], fb["tu"], ALU.mult, [fb["tg_tok"], fb["tu_tok"]], [fb["a_tok"][jj]])
                    for c in range(NK):
                        wb, wtok = load_w(("dn", c, ps_))
                        pt, ptok = psA.get()
                        for k in range(11):
                            mm(pt[:, :], wb[:, k, :], actT[:, k, :], k == 0, k == 10, [wtok, fb["a_tok"][k]], [ptok])
                        tt("dve", x_sb[:, c, :], x_sb[:, c, :], pt[:, :], ALU.add, [ptok, x_tok[c]], [x_tok[c]])

                chk(7)
                if last and do_final:
                    pst, pstok = psC.get()
                    for c in range(NK):
                        sq, sqt = sq_ring.get()
                        act(sq[:], x_sb[:, c, :], AF.Square, [x_tok[c]], [sqt])
                        mm(pst[:, :], ones_f[:], sq[:], c == 0, c == NK - 1, [sqt, c_tok], [pstok])
                    ts("dve", rstd[:], pst[:, :], 1.0 / D, EPS, ALU.mult, ALU.add, [pstok], [rstd_tok])
                    act(rstd[:], rstd[:], AF.Sqrt, [rstd_tok], [rstd_tok])
                    P.op("dve", lambda h: h.reciprocal(rstd[:], rstd[:]), [rstd_tok], [rstd_tok])
                    for c in range(NK):
                        stt(x_sb[:, c, :], x_sb[:, c, :], gains[:, 2 * L + 1, c:c + 1], rstd[:], ALU.mult, ALU.mult,
                            [x_tok[c], rstd_tok, c_tok], [x_tok[c]])
                dst_d = out_d if last else xs_d
                dma("act", dst_d[:, t0:t0 + TW].rearrange("(c p) t -> p c t", p=128), x_sb[:], x_tok, [xs_tok])

    except _Stop:
        pass
    P.emit()
    es.close()
    return nc


cst_cdec = make_consts()["cdec"]
INPUT_NAMES = (
    "x",
    "mem",
    "mem_norm",
    "l0_norm_mix",
    "l0_w_in",
    "l0_ret_gn",
    "l0_w_mem_kv",
    "l0_w_o",
    "l0_norm_ffn",
    "l0_ffn_w_up",
    "l0_ffn_conv_w",
    "l0_ffn_conv_b",
    "l0_ffn_w_down",
    "l1_norm_mix",
    "l1_w_in",
    "l1_conv_w",
    "l1_w_mem_kv",
    "l1_w_o",
    "l1_norm_ffn",
    "l1_ffn_w_up",
    "l1_ffn_conv_w",
    "l1_ffn_conv_b",
    "l1_ffn_w_down",
    "l2_norm_mix",
    "l2_w_in",
    "l2_w_mem_kv",
    "l2_w_o",
    "l2_norm_ffn",
    "l2_ffn_w_up",
    "l2_ffn_conv_w",
    "l2_ffn_conv_b",
    "l2_ffn_w_down",
    "l3_norm_mix",
    "l3_w_in",
    "l3_ret_gn",
    "l3_w_mem_kv",
    "l3_w_o",
    "l3_norm_ffn",
    "l3_ffn_w_up",
    "l3_ffn_conv_w",
    "l3_ffn_conv_b",
    "l3_ffn_w_down",
    "final_norm",
)
_CACHE = {}


def host_inputs(inputs, L=4):
    c = make_consts()
    shared = {}
    for k in CONST_SHAPES:
        shared["c_" + k] = np.ascontiguousarray(c[k], dtype=np.float32)
    g = [vec16(inputs["mem_norm"])]
    for li in range(L):
        g.append(vec16(inputs["l%d_norm_mix" % li]))
        g.append(vec16(inputs["l%d_norm_ffn" % li]))
    g.append(vec16(inputs["final_norm"]))
    shared["gains"] = np.ascontiguousarray(np.stack(g, axis=1))
    for li in range(L):
        kind = KINDS[li]
        p = "l%d_" % li
        shared[p + "w_in"] = wchunks(inputs[p + "w_in"])
        shared[p + "w_kv"] = wchunks(inputs[p + "w_mem_kv"])
        shared[p + "w_o"] = wchunks(inputs[p + "w_o"])
        shared[p + "w_up"] = wchunks(inputs[p + "ffn_w_up"])
        wd = wchunks(inputs[p + "ffn_w_down"])
        shared[p + "w_dn"] = np.ascontiguousarray(wd.reshape(16, 128, 4, 11, 128).transpose(0, 2, 1, 3, 4))
        cw = np.asarray(inputs[p + "ffn_conv_w"], np.float32)
        shared[p + "cw"] = np.ascontiguousarray(cw.reshape(3, 88, 128).transpose(2, 0, 1))
        cb = np.asarray(inputs[p + "ffn_conv_b"], np.float32)
        shared[p + "cb"] = np.ascontiguousarray(cb.reshape(88, 128).T)
        if kind == 0:
            shared[p + "gn"] = np.ascontiguousarray(np.asarray(inputs[p + "ret_gn"], np.float32).reshape(12, 128).T)
        if kind == 1:
            sc = np.asarray(inputs[p + "conv_w"], np.float32)
            shared[p + "scw"] = np.ascontiguousarray(sc.reshape(3, 12, 128).transpose(2, 0, 1))
    return shared


def kernel(**inputs):
    for nm in INPUT_NAMES:
        assert nm in inputs, nm
    x = np.asarray(inputs["x"], np.float32)
    mem = np.asarray(inputs["mem"], np.float32)
    B, S, _ = x.shape
    key = (S,)
    if key not in _CACHE:
        _CACHE[key] = build(S)
    nc = _CACHE[key]
    shared = host_inputs(inputs)
    in_maps = []
    for cidx in range(8):
        b = cidx % B
        m = dict(shared)
        m["xT"] = np.ascontiguousarray(x[b].T)
        m["memT"] = np.ascontiguousarray(mem[b].T)
        in_maps.append(m)
    res = run_bass_kernel_spmd(nc, in_maps, core_ids=list(range(8)))
    out = np.stack([np.ascontiguousarray(res.results[b]["outT"].T) for b in range(B)], axis=0)
    return out.astype(np.float32)
```
